# Optimizing a Trainium2 kernel written in Bass

```python
import math
import jax
import jax.numpy as jnp
from jax import lax
import numpy as np

D_MODEL = 2048
BATCH = 2
SEQ = 4096
DEPTH = 1

MEM_LEN = 256
SSD_D_INNER = D_MODEL
SSD_HEAD_DIM = 64
SSD_HEADS = SSD_D_INNER // SSD_HEAD_DIM
SSD_GROUPS = 4
SSD_STATE = 128
SSD_CONV = 4
SSD_CHUNK = 128
SSD_CONV_DIM = SSD_D_INNER + 2 * SSD_GROUPS * SSD_STATE
SWA_HEADS = 16
SWA_KV_HEADS = 4
SWA_HEAD_DIM = 64
SWA_WINDOW = 128
SWA_BLOCK = SWA_WINDOW
REL_BUCKETS = 32
REL_MAX_DIST = 128
XA_HEADS = 4
XA_HEAD_DIM = D_MODEL // 8
N_BRANCH = 3
N_EXPERTS = 64
TOP_K = 8
N_EXPERT_GROUPS = 8
TOPK_GROUPS = 4
EXPERT_DIM = D_MODEL // 4
SHARED_DIM = D_MODEL // 4
ROUTED_SCALE = 2.5
MOE_BLOCK = 128
ALPHA = (2.0 * DEPTH) ** 0.25
BETA = (8.0 * DEPTH) ** -0.25
LN_EPS = 1e-5
RMS_EPS = 1e-5

SWA_Q_DIM = SWA_HEADS * SWA_HEAD_DIM
SWA_KV_DIM = SWA_KV_HEADS * SWA_HEAD_DIM
XA_DIM = XA_HEADS * XA_HEAD_DIM
IN_SIZES = (SSD_D_INNER, SSD_CONV_DIM, SSD_HEADS, SWA_Q_DIM, SWA_KV_DIM, SWA_KV_DIM, XA_DIM, N_BRANCH * D_MODEL)
IN_TOTAL = sum(IN_SIZES)

kernel_name = "hybrid_ssd_swa_memxattn_moe_deepnorm"


def _split_points(sizes):
    pts, acc = [], 0
    for s in sizes[:-1]:
        acc += s
        pts.append(acc)
    return pts


def layer_norm(x, g, b):
    xf = x.astype(jnp.float32)
    mu = jnp.mean(xf, axis=-1, keepdims=True)
    var = jnp.mean(jnp.square(xf - mu), axis=-1, keepdims=True)
    return ((xf - mu) * lax.rsqrt(var + LN_EPS)).astype(x.dtype) * g + b


def causal_dwconv(u, w, b):
    c = u.shape[-1]
    out = lax.conv_general_dilated(u, w[:, None, :], window_strides=(1,), padding=[(SSD_CONV - 1, 0)],
                                   dimension_numbers=('NWC', 'WIO', 'NWC'), feature_group_count=c)
    return out + b


def ssd_chunked_scan(xs, dt, a, bm, cm):
    b, l, nh, p = xs.shape
    g, n = bm.shape[2], bm.shape[3]
    e = nh // g
    q = SSD_CHUNK
    nc = l // q
    dtype = xs.dtype
    xd = (xs * dt.astype(dtype)[..., None]).reshape(b, nc, q, g, e, p)
    la_cum = jnp.cumsum((dt * a).reshape(b, nc, q, g, e), axis=2)
    bc = bm.reshape(b, nc, q, g, n)
    cc = cm.reshape(b, nc, q, g, n)
    seg = la_cum[:, :, :, None] - la_cum[:, :, None, :]
    causal = jnp.tril(jnp.ones((q, q), dtype=bool))[:, :, None, None]
    decay = jnp.exp(jnp.where(causal, seg, -jnp.inf)).astype(dtype)
    cb = jnp.einsum('bcign,bcjgn->bcijg', cc, bc)
    y_diag = jnp.einsum('bcijg,bcijge,bcjgep->bcigep', cb, decay, xd)
    decay_end = jnp.exp(la_cum[:, :, -1:] - la_cum).astype(dtype)
    states = jnp.einsum('bcjgn,bcjge,bcjgep->bcgepn', bc, decay_end, xd).astype(jnp.float32)
    chunk_decay = jnp.exp(la_cum[:, :, -1])

    def step(h, inp):
        s_c, d_c = inp
        return h * d_c[..., None, None] + s_c, h

    h0 = jnp.zeros((b, g, e, p, n), jnp.float32)
    _, h_prev = lax.scan(step, h0, (jnp.moveaxis(states, 1, 0), jnp.moveaxis(chunk_decay, 1, 0)))
    h_prev = jnp.moveaxis(h_prev, 0, 1).astype(dtype)
    y_off = jnp.einsum('bcign,bcgepn,bcige->bcigep', cc, h_prev, jnp.exp(la_cum).astype(dtype))
    return (y_diag + y_off).reshape(b, l, nh, p)


def ssd_mixer(z, xbc, dt_raw, conv_w, conv_b, dt_bias, a_log, d_skip, norm_g):
    b, l, _ = z.shape
    xbc = jax.nn.silu(causal_dwconv(xbc, conv_w, conv_b))
    xs, bm, cm = jnp.split(xbc, [SSD_D_INNER, SSD_D_INNER + SSD_GROUPS * SSD_STATE], axis=-1)
    xs = xs.reshape(b, l, SSD_HEADS, SSD_HEAD_DIM)
    bm = bm.reshape(b, l, SSD_GROUPS, SSD_STATE)
    cm = cm.reshape(b, l, SSD_GROUPS, SSD_STATE)
    dt = jax.nn.softplus((dt_raw + dt_bias).astype(jnp.float32))
    a = -jnp.exp(a_log.astype(jnp.float32))
    y = ssd_chunked_scan(xs, dt, a, bm, cm) + xs * d_skip[:, None]
    y = y.reshape(b, l, SSD_D_INNER) * jax.nn.silu(z)
    yg = y.reshape(b, l, SSD_GROUPS, -1).astype(jnp.float32)
    yg = yg * lax.rsqrt(jnp.mean(jnp.square(yg), axis=-1, keepdims=True) + RMS_EPS)
    return yg.reshape(b, l, SSD_D_INNER).astype(z.dtype) * norm_g


def t5_causal_bucket(dist):
    max_exact = REL_BUCKETS // 2
    large = max_exact + (jnp.log(jnp.maximum(dist, 1).astype(jnp.float32) / max_exact)
                         / math.log(REL_MAX_DIST / max_exact) * (REL_BUCKETS - max_exact)).astype(jnp.int32)
    large = jnp.minimum(large, REL_BUCKETS - 1)
    return jnp.where(dist < max_exact, dist, large)


def swa_mixer(q, k, v, sinks, rel_bias):
    b, l, _ = q.shape
    nb = l // SWA_BLOCK
    grp = SWA_HEADS // SWA_KV_HEADS
    q = q.reshape(b, nb, SWA_BLOCK, SWA_KV_HEADS, grp, SWA_HEAD_DIM)
    pad = jnp.zeros((b, SWA_BLOCK, SWA_KV_HEADS, SWA_HEAD_DIM), k.dtype)
    kp = jnp.concatenate([pad, k.reshape(b, l, SWA_KV_HEADS, SWA_HEAD_DIM)], axis=1)
    vp = jnp.concatenate([pad, v.reshape(b, l, SWA_KV_HEADS, SWA_HEAD_DIM)], axis=1)
    kp = kp.reshape(b, nb + 1, SWA_BLOCK, SWA_KV_HEADS, SWA_HEAD_DIM)
    vp = vp.reshape(b, nb + 1, SWA_BLOCK, SWA_KV_HEADS, SWA_HEAD_DIM)
    kband = jnp.concatenate([kp[:, :-1], kp[:, 1:]], axis=2)
    vband = jnp.concatenate([vp[:, :-1], vp[:, 1:]], axis=2)
    scores = jnp.einsum('bnihgd,bnjhd->bnhgij', q, kband).astype(jnp.float32) * (SWA_HEAD_DIM ** -0.5)
    qi = jnp.arange(SWA_BLOCK)[:, None]
    kj = jnp.arange(2 * SWA_BLOCK)[None, :]
    dist = qi + SWA_BLOCK - kj
    in_window = (dist >= 0) & (dist < SWA_WINDOW)
    block_valid = (jnp.arange(nb)[:, None, None] > 0) | (kj[None] >= SWA_BLOCK)
    mask = in_window[None] & block_valid
    bias = rel_bias.astype(jnp.float32)[t5_causal_bucket(jnp.maximum(dist, 0))]
    bias = bias.transpose(2, 0, 1).reshape(SWA_KV_HEADS, grp, SWA_BLOCK, 2 * SWA_BLOCK)
    scores = jnp.where(mask[None, :, None, None], scores + bias, -jnp.inf)
    sink = sinks.astype(jnp.float32).reshape(SWA_KV_HEADS, grp)[:, :, None, None]
    m = jnp.maximum(jnp.max(scores, axis=-1, keepdims=True), sink)
    pr = jnp.exp(scores - m)
    pr = pr / (jnp.sum(pr, axis=-1, keepdims=True) + jnp.exp(sink - m))
    out = jnp.einsum('bnhgij,bnjhd->bnihgd', pr.astype(vband.dtype), vband)
    return out.reshape(b, l, SWA_Q_DIM)


def mem_cross_attention(q, mem_kv):
    b, l, _ = q.shape
    q = q.reshape(b, l, XA_HEADS, XA_HEAD_DIM)
    mk, mv = jnp.split(mem_kv, 2, axis=-1)
    mk = mk.reshape(b, -1, XA_HEADS, XA_HEAD_DIM)
    mv = mv.reshape(b, -1, XA_HEADS, XA_HEAD_DIM)
    s = jnp.einsum('blhd,bmhd->bhlm', q, mk).astype(jnp.float32) * (XA_HEAD_DIM ** -0.5)
    pr = jax.nn.softmax(s, axis=-1).astype(mv.dtype)
    return jnp.einsum('bhlm,bmhd->blhd', pr, mv).reshape(b, l, XA_DIM)


def mixer_sublayer(h, mem, w_in, conv_w, conv_b, dt_bias, a_log, d_skip, ssd_norm_g, swa_sinks, rel_bias,
                   w_mem_kv, w_ssd_o, w_swa_o, w_xa_o, w_out):
    b, l, _ = h.shape
    proj = h @ w_in
    z, xbc, dt_raw, q_s, k_s, v_s, q_x, gates = jnp.split(proj, _split_points(IN_SIZES), axis=-1)
    y_ssd = ssd_mixer(z, xbc, dt_raw, conv_w, conv_b, dt_bias, a_log, d_skip, ssd_norm_g) @ w_ssd_o
    y_swa = swa_mixer(q_s, k_s, v_s, swa_sinks, rel_bias) @ w_swa_o
    y_xa = mem_cross_attention(q_x, mem @ w_mem_kv) @ w_xa_o
    g = jax.nn.sigmoid(gates.astype(jnp.float32)).astype(h.dtype).reshape(b, l, N_BRANCH, D_MODEL)
    merged = g[:, :, 0] * y_ssd + g[:, :, 1] * y_swa + g[:, :, 2] * y_xa
    return merged @ w_out


def route(xf, router_w, router_bias):
    t = xf.shape[0]
    scores = jax.nn.sigmoid((xf @ router_w).astype(jnp.float32))
    sel = scores + router_bias.astype(jnp.float32)
    grp = sel.reshape(t, N_EXPERT_GROUPS, N_EXPERTS // N_EXPERT_GROUPS)
    grp_score = jnp.sum(lax.top_k(grp, 2)[0], axis=-1)
    _, top_g = lax.top_k(grp_score, TOPK_GROUPS)
    gmask = jnp.sum(jax.nn.one_hot(top_g, N_EXPERT_GROUPS, dtype=jnp.float32), axis=1) > 0
    gmask = jnp.repeat(gmask, N_EXPERTS // N_EXPERT_GROUPS, axis=-1)
    _, idx = lax.top_k(jnp.where(gmask, sel, -jnp.inf), TOP_K)
    w = jnp.take_along_axis(scores, idx, axis=-1)
    w = w / jnp.sum(w, axis=-1, keepdims=True) * ROUTED_SCALE
    return idx, w


def routed_experts(xf, idx, w, w1, w3, w2):
    t = xf.shape[0]
    tk = t * TOP_K
    flat_e = idx.reshape(-1)
    order = jnp.argsort(flat_e)
    sorted_e = flat_e[order]
    counts = jnp.bincount(flat_e, length=N_EXPERTS)
    padded = (counts + MOE_BLOCK - 1) // MOE_BLOCK * MOE_BLOCK
    start = jnp.cumsum(counts) - counts
    pend = jnp.cumsum(padded)
    pstart = pend - padded
    dest = pstart[sorted_e] + jnp.arange(tk) - start[sorted_e]
    n_blocks = -(-tk // MOE_BLOCK) + N_EXPERTS
    n_slots = n_blocks * MOE_BLOCK
    slot_tok = jnp.full((n_slots,), t, jnp.int32).at[dest].set((order // TOP_K).astype(jnp.int32))
    slot_w = jnp.zeros((n_slots,), w.dtype).at[dest].set(w.reshape(-1)[order])
    block_e = jnp.minimum(jnp.searchsorted(pend, jnp.arange(n_blocks) * MOE_BLOCK, side='right'), N_EXPERTS - 1)
    xpad = jnp.concatenate([xf, jnp.zeros((1, xf.shape[1]), xf.dtype)], axis=0)

    def block_ffn(args):
        tok, wt, e = args
        xb = xpad[tok]
        hdn = jax.nn.silu(xb @ w1[e]) * (xb @ w3[e])
        return (hdn @ w2[e]) * wt[:, None].astype(xb.dtype)

    yb = lax.map(block_ffn, (slot_tok.reshape(n_blocks, MOE_BLOCK), slot_w.reshape(n_blocks, MOE_BLOCK), block_e))
    out = jnp.zeros((t + 1, xf.shape[1]), xf.dtype).at[slot_tok].add(yb.reshape(n_slots, -1))
    return out[:t]


def moe_sublayer(h, router_w, router_bias, w1, w3, w2, ws1, ws3, ws2):
    b, l, d = h.shape
    xf = h.reshape(b * l, d)
    idx, w = route(xf, router_w, router_bias)
    y = routed_experts(xf, idx, w, w1, w3, w2) + (jax.nn.silu(xf @ ws1) * (xf @ ws3)) @ ws2
    return y.reshape(b, l, d)


def setup_inputs(seed: int = 0) -> dict:
    key = jax.random.key(seed)
    ks = jax.random.split(key, 32)
    f32 = jnp.float32
    nrm = lambda k, shape, s: jax.random.normal(k, shape, f32) * s
    dt0 = jnp.exp(jax.random.uniform(ks[5], (DEPTH, SSD_HEADS), f32, math.log(1e-3), math.log(1e-1)))
    return {
        "x": nrm(ks[0], (BATCH, SEQ, D_MODEL), 1.0),
        "mem": nrm(ks[1], (BATCH, MEM_LEN, D_MODEL), 1.0),
        "w_in": nrm(ks[2], (DEPTH, D_MODEL, IN_TOTAL), D_MODEL ** -0.5),
        "conv_w": nrm(ks[3], (DEPTH, SSD_CONV, SSD_CONV_DIM), SSD_CONV ** -0.5),
        "conv_b": nrm(ks[4], (DEPTH, SSD_CONV_DIM), 0.01),
        "dt_bias": dt0 + jnp.log(-jnp.expm1(-dt0)),
        "a_log": jnp.log(jax.random.uniform(ks[6], (DEPTH, SSD_HEADS), f32, 1.0, 16.0)),
        "d_skip": 1.0 + nrm(ks[7], (DEPTH, SSD_HEADS), 0.01),
        "ssd_norm_g": 1.0 + nrm(ks[8], (DEPTH, SSD_D_INNER), 0.01),
        "swa_sinks": nrm(ks[9], (DEPTH, SWA_HEADS), 1.0),
        "rel_bias": nrm(ks[10], (REL_BUCKETS, SWA_HEADS), 0.5),
        "w_mem_kv": nrm(ks[11], (DEPTH, D_MODEL, 2 * XA_DIM), D_MODEL ** -0.5),
        "w_ssd_o": nrm(ks[12], (DEPTH, SSD_D_INNER, D_MODEL), BETA * SSD_D_INNER ** -0.5),
        "w_swa_o": nrm(ks[13], (DEPTH, SWA_Q_DIM, D_MODEL), BETA * SWA_Q_DIM ** -0.5),
        "w_xa_o": nrm(ks[14], (DEPTH, XA_DIM, D_MODEL), BETA * XA_DIM ** -0.5),
        "w_out": nrm(ks[15], (DEPTH, D_MODEL, D_MODEL), BETA * D_MODEL ** -0.5),
        "ln1_g": 1.0 + nrm(ks[16], (DEPTH, D_MODEL), 0.01),
        "ln1_b": nrm(ks[17], (DEPTH, D_MODEL), 0.01),
        "router_w": nrm(ks[18], (DEPTH, D_MODEL, N_EXPERTS), D_MODEL ** -0.5),
        "router_bias": nrm(ks[19], (DEPTH, N_EXPERTS), 0.01),
        "w1": nrm(ks[20], (DEPTH, N_EXPERTS, D_MODEL, EXPERT_DIM), D_MODEL ** -0.5),
        "w3": nrm(ks[21], (DEPTH, N_EXPERTS, D_MODEL, EXPERT_DIM), D_MODEL ** -0.5),
        "w2": nrm(ks[22], (DEPTH, N_EXPERTS, EXPERT_DIM, D_MODEL), BETA * EXPERT_DIM ** -0.5),
        "ws1": nrm(ks[23], (DEPTH, D_MODEL, SHARED_DIM), D_MODEL ** -0.5),
        "ws3": nrm(ks[24], (DEPTH, D_MODEL, SHARED_DIM), D_MODEL ** -0.5),
        "ws2": nrm(ks[25], (DEPTH, SHARED_DIM, D_MODEL), BETA * SHARED_DIM ** -0.5),
        "ln2_g": 1.0 + nrm(ks[26], (DEPTH, D_MODEL), 0.01),
        "ln2_b": nrm(ks[27], (DEPTH, D_MODEL), 0.01),
    }


def reference(x, mem, w_in, conv_w, conv_b, dt_bias, a_log, d_skip, ssd_norm_g, swa_sinks, rel_bias,
              w_mem_kv, w_ssd_o, w_swa_o, w_xa_o, w_out, ln1_g, ln1_b, router_w, router_bias,
              w1, w3, w2, ws1, ws3, ws2, ln2_g, ln2_b):
    h = x
    for i in range(DEPTH):
        mix = mixer_sublayer(h, mem, w_in[i], conv_w[i], conv_b[i], dt_bias[i], a_log[i], d_skip[i],
                             ssd_norm_g[i], swa_sinks[i], rel_bias, w_mem_kv[i], w_ssd_o[i], w_swa_o[i],
                             w_xa_o[i], w_out[i])
        h = layer_norm(ALPHA * h + mix, ln1_g[i], ln1_b[i])
        ffn = moe_sublayer(h, router_w[i], router_bias[i], w1[i], w3[i], w2[i], ws1[i], ws3[i], ws2[i])
        h = layer_norm(ALPHA * h + ffn, ln2_g[i], ln2_b[i])
    return h
```

```python
from contextlib import ExitStack
import math
import numpy as np
import concourse.bass as bass
import concourse.mybir as mybir
from concourse.bass_utils import run_bass_kernel_spmd

F32 = mybir.dt.float32
BF16 = mybir.dt.bfloat16
I32 = mybir.dt.int32
U32 = mybir.dt.uint32
AF = mybir.ActivationFunctionType
ALU = mybir.AluOpType
AX = mybir.AxisListType

ENGS = ("pe", "act", "dve", "pool", "sp")
NEG = -30000.0


class Cfg:
    def __init__(self, D=2048, T=1024, W=4096, HQ=16, HKV=4, XAH=4, MEM=256, CAP=256, NCORES=8, BATCH=2):
        self.D = D
        self.T = T
        self.W = W
        self.KC = D // 128
        self.H = D // 64
        self.G = 4
        self.HG = self.H // 4
        self.GW = D // 4
        self.N = 128
        self.NCX = D // 128
        self.NCC = self.NCX + 8
        self.CONV = D + 1024
        self.HQ = HQ
        self.HKV = HKV
        self.GRP = HQ // HKV
        self.QD = HQ * 64
        self.KVD = HKV * 64
        self.XAH = XAH
        self.XAD = D // 8
        self.XD = XAH * self.XAD
        self.XDC = self.XAD // 128
        self.MEM = MEM
        self.E = 64
        self.ED = D // 4
        self.SD = D // 4
        self.CAP = CAP
        self.NB = T // 128
        self.NW = W // 128
        self.NP = self.NW - self.NB
        sizes = (D, self.CONV, self.H, self.QD, self.KVD, self.KVD, self.XD, 3 * D)
        offs = [0]
        for s in sizes:
            offs.append(offs[-1] + s)
        (self.o_z, self.o_xbc, self.o_dt, self.o_q, self.o_k, self.o_v, self.o_qx, self.o_g, self.IN) = offs
        self.NCORES = NCORES
        self.BATCH = BATCH
        self.CPS = NCORES // BATCH
        self.ALPHA = 2.0 ** 0.25
        self.LN_EPS = 1e-5
        self.RMS_EPS = 1e-5


class Res:
    __slots__ = ("name", "w", "r")

    def __init__(self, name):
        self.name = name
        self.w = None
        self.r = {}


class Op:
    __slots__ = ("eng", "fn", "deps", "pos", "signal", "sigval", "is_dma", "dsem", "dval",
                 "waits", "flushed", "uid")


class Prog:
    def __init__(self, nc, es, n_dma_sems=24):
        self.nc = nc
        self.esem = {e: es.enter_context(nc.semaphore("s_" + e)) for e in ENGS}
        self.dsems = [es.enter_context(nc.semaphore("d%d" % i)) for i in range(n_dma_sems)]
        self.dval = [0] * n_dma_sems
        self.dlast = [None] * n_dma_sems
        self.dnext = 0
        self.dnext_sw = 0
        self.pending = {e: [] for e in ENGS}
        self.pos = {e: 0 for e in ENGS}
        self.sigcnt = {e: 0 for e in ENGS}
        self.seen = {e: {} for e in ENGS}
        self.seen_d = {e: {} for e in ENGS}
        self.uid = 0
        self.nops = 0

    def _mk(self, eng, fn, reads, writes, is_dma):
        o = Op()
        o.eng = eng
        o.fn = fn
        o.is_dma = is_dma
        o.signal = False
        o.sigval = 0
        o.flushed = False
        o.waits = None
        self.uid += 1
        o.uid = self.uid
        deps = {}
        for r in reads:
            if r.w is not None:
                deps[r.w.uid] = r.w
        for w in writes:
            if w.w is not None:
                deps[w.w.uid] = w.w
            for rd in w.r.values():
                deps[rd.uid] = rd
        o.deps = [d for d in deps.values() if not d.flushed]
        key = ("d", o.uid) if is_dma else eng
        for r in reads:
            r.r[key] = o
        for w in writes:
            w.w = o
            w.r = {}
        self.pos[eng] += 1
        o.pos = self.pos[eng]
        self.pending[eng].append(o)
        return o

    def op(self, eng, fn, reads=(), writes=()):
        return self._mk(eng, fn, reads, writes, False)

    def dma(self, eng, fn, reads=(), writes=(), n=1):
        o = self._mk(eng, fn, reads, writes, True)
        half = len(self.dsems) // 2
        if eng == "pool":
            s = half + self.dnext_sw
            self.dnext_sw = (self.dnext_sw + 1) % (len(self.dsems) - half)
        else:
            s = self.dnext
            self.dnext = (self.dnext + 1) % half
        prev = self.dlast[s]
        if prev is not None and not prev.flushed:
            o.deps.append(prev)
        self.dval[s] += 16 * n
        o.dsem = s
        o.dval = self.dval[s]
        self.dlast[s] = o
        return o

    def flush(self):
        nc = self.nc
        for e in ENGS:
            seen = self.seen[e]
            seen_d = self.seen_d[e]
            for o in self.pending[e]:
                waits = []
                for d in o.deps:
                    if d.flushed:
                        continue
                    if d.is_dma:
                        if seen_d.get(d.dsem, 0) >= d.dval:
                            continue
                        seen_d[d.dsem] = d.dval
                        waits.append(d)
                    else:
                        if d.eng == e and e == "pe":
                            continue
                        if seen.get(d.eng, 0) >= d.pos:
                            continue
                        seen[d.eng] = d.pos
                        d.signal = True
                        waits.append(d)
                o.waits = waits
        endops = {}
        for e in ENGS:
            for o in reversed(self.pending[e]):
                if not o.is_dma:
                    o.signal = True
                    endops[e] = o
                    break
        for e in ENGS:
            for o in self.pending[e]:
                if o.signal and not o.is_dma:
                    self.sigcnt[e] += 1
                    o.sigval = self.sigcnt[e]
        esem = self.esem
        dsems = self.dsems
        dfinal = [(i, self.dval[i]) for i in range(len(dsems))
                  if self.dlast[i] is not None and not self.dlast[i].flushed]
        pending = self.pending

        def emit(e, eng):
            for o in pending[e]:
                for d in o.waits:
                    if d.is_dma:
                        eng.wait_ge(dsems[d.dsem], d.dval)
                    else:
                        eng.wait_ge(esem[d.eng], d.sigval)
                if o.is_dma:
                    for ins in o.fn(eng):
                        ins.then_inc(dsems[o.dsem], 16)
                else:
                    ins = o.fn(eng)
                    if o.signal:
                        ins.then_inc(esem[e], 1)
            for f in ENGS:
                if f != e and f in endops:
                    eng.wait_ge(esem[f], endops[f].sigval)
            for si, v in dfinal:
                eng.wait_ge(dsems[si], v)

        with nc.Block() as block:
            @block.tensor
            def _(eng):
                emit("pe", eng)

            @block.scalar
            def _(eng):
                emit("act", eng)

            @block.vector
            def _(eng):
                emit("dve", eng)

            @block.gpsimd
            def _(eng):
                emit("pool", eng)

            @block.sync
            def _(eng):
                emit("sp", eng)
        for e in ENGS:
            self.nops += len(self.pending[e])
            for o in self.pending[e]:
                o.flushed = True
                o.fn = None
                o.deps = None
            self.pending[e] = []


class Tl:
    __slots__ = ("t", "r", "rs")

    def __init__(self, t, name, nres=0):
        self.t = t
        self.r = Res(name)
        self.rs = [Res(name + str(i)) for i in range(nres)]


class KB:
    def __init__(self, nc, cfg, es):
        self.nc = nc
        self.c = cfg
        self.P = Prog(nc, es)
        self.ps = es.enter_context(nc.psum_tensor("PS", [128, 4096], F32))
        self.pr = [Res("ps%d" % i) for i in range(8)]

    def bank(self, i, n=512, off=0):
        return self.ps[:, i * 512 + off:i * 512 + off + n]

    def bankbf(self, i):
        return self.ps[:, i * 512:(i + 1) * 512].bitcast(BF16)

    def mm(self, out, lhsT, rhs, start, stop, R, W):
        return self.P.op("pe", lambda e: e.matmul(out, lhsT=lhsT, rhs=rhs, start=start, stop=stop), R, W)

    def tr(self, out, in_, ident, R, W):
        return self.P.op("pe", lambda e: e.transpose(out, in_, ident), R, W)

    def act(self, out, in_, func, R, W, bias=None, scale=1.0, accum=None):
        kw = {}
        if bias is not None:
            kw["bias"] = bias
        if accum is not None:
            kw["accum_out"] = accum
        return self.P.op("act", lambda e: e.activation(out=out, in_=in_, func=func, scale=scale, **kw), R, W)

    def tt(self, out, in0, in1, op, R, W, eng="dve"):
        return self.P.op(eng, lambda e: e.tensor_tensor(out=out, in0=in0, in1=in1, op=op), R, W)

    def ts(self, out, in0, s1, s2, op0, op1, R, W, accum=None, eng="dve"):
        kw = {}
        if accum is not None:
            kw["accum_out"] = accum
        if op1 is None:
            return self.P.op(eng, lambda e: e.tensor_scalar(out=out, in0=in0, scalar1=s1, scalar2=None, op0=op0, **kw), R, W)
        return self.P.op(eng, lambda e: e.tensor_scalar(out=out, in0=in0, scalar1=s1, scalar2=s2, op0=op0, op1=op1, **kw), R, W)

    def stt(self, out, in0, scalar, in1, op0, op1, R, W, eng="dve"):
        return self.P.op(eng, lambda e: e.scalar_tensor_tensor(out=out, in0=in0, scalar=scalar, in1=in1, op0=op0, op1=op1), R, W)

    def cp(self, out, in_, R, W, eng="dve"):
        if eng == "act":
            return self.P.op("act", lambda e: e.copy(out=out, in_=in_), R, W)
        return self.P.op(eng, lambda e: e.tensor_copy(out=out, in_=in_), R, W)

    def red(self, out, in_, op, R, W, eng="dve"):
        return self.P.op(eng, lambda e: e.tensor_reduce(out=out, in_=in_, axis=AX.X, op=op), R, W)

    def memset(self, out, val, R, W, eng="dve"):
        return self.P.op(eng, lambda e: e.memset(out, val), R, W)

    def dma(self, out, in_, R, W, eng="sp"):
        return self.P.dma(eng, lambda e: [e.dma_start(out=out, in_=in_)], R, W)

    def scatter(self, out, offs, in_, bound, R, W):
        return self.P.dma("pool", lambda e: [e.indirect_dma_start(
            out=out, out_offset=bass.IndirectOffsetOnAxis(ap=offs, axis=0), in_=in_, in_offset=None,
            bounds_check=bound, oob_is_err=False)], R, W)

    def gather(self, out, in_, offs, bound, R, W):
        return self.P.dma("pool", lambda e: [e.indirect_dma_start(
            out=out, out_offset=None, in_=in_, in_offset=bass.IndirectOffsetOnAxis(ap=offs, axis=0),
            bounds_check=bound, oob_is_err=False)], R, W)


_UNIQ = [0]


def _sbf(nc, stack):
    def sb(name, shape, dt, nres=0):
        _UNIQ[0] += 1
        name = "%s_%d" % (name, _UNIQ[0])
        return Tl(stack.enter_context(nc.sbuf_tensor(name, shape, dt)), name, nres)
    return sb


def load_xm(kb, io, sb):
    c = kb.c
    xm = sb("xm", [128, c.KC, 128 + c.T], BF16, c.KC)
    lo = 3 + c.W - c.T - 128
    for kc in range(c.KC):
        kb.dma(xm.t[:, kc, :], io["xTw"][kc * 128:(kc + 1) * 128, lo:lo + 128 + c.T], [], [xm.rs[kc]], eng="pool")
    return xm


def phase1(kb, io, S):
    c = kb.c
    nc = kb.nc
    W, T, KC = c.W, c.T, c.KC
    TT = 256
    with ExitStack() as st:
        sb = _sbf(nc, st)
        wall = sb("wall", [128, KC, c.CONV], BF16, c.NCC)
        for cc in range(c.NCC):
            col0 = c.o_xbc + cc * 128
            kb.dma(wall.t[:, :, cc * 128:(cc + 1) * 128], io["w_in"][:, col0:col0 + 128].rearrange("(kc p) n -> p kc n", p=128),
                   [], [wall.rs[cc]], eng="pool")
        cw = sb("cw", [128, c.NCC, 4], F32)
        cbt = sb("cbt", [128, c.NCC], F32)
        kb.dma(cw.t[:], io["conv_wT"], [], [cw.r])
        kb.dma(cbt.t[:], io["conv_bT"], [], [cbt.r])
        xw = [sb("xw%d" % i, [128, KC, TT + 3], BF16) for i in range(2)]
        raw = [sb("raw%d" % i, [128, TT + 3], F32) for i in range(2)]
        acc = [sb("acc%d" % i, [128, TT], F32) for i in range(2)]
        feat = [sb("feat%d" % i, [128, TT], BF16) for i in range(2)]
        sgx = [sb("sgx%d" % i, [128, 2, c.D], BF16) for i in range(2)]
        sgb = [sb("sgb%d" % i, [128, 2, 512], BF16) for i in range(2)]
        ident = S["ident"]
        it = 0
        for tt in range(W // TT):
            t0 = tt * TT
            ismain = t0 >= W - T
            x_ = xw[tt % 2]
            kb.dma(x_.t[:], io["xTw"][:, t0:t0 + TT + 3].rearrange("(kc p) n -> p kc n", p=128), [], [x_.r], eng="pool")
            sx = sgx[tt % 2]
            sb_ = sgb[tt % 2]
            for cc in range(c.NCC):
                isC = cc >= c.NCX + 4
                if isC and not ismain:
                    continue
                b = it % 4
                rw = raw[it % 2]
                a_ = acc[it % 2]
                ft = feat[it % 2]
                tb = 4 + it % 4
                it += 1
                for kc in range(KC):
                    kb.mm(kb.bank(b, TT + 3), wall.t[:, kc, cc * 128:(cc + 1) * 128], x_.t[:, kc, :], kc == 0, kc == KC - 1,
                          [wall.rs[cc], x_.r], [kb.pr[b]])
                kb.cp(rw.t[:], kb.bank(b, TT + 3), [kb.pr[b]], [rw.r], eng="act")
                kb.ts(a_.t[:], rw.t[:, 0:TT], cw.t[:, cc, 0:1], None, ALU.mult, None, [rw.r, cw.r], [a_.r])
                for k in range(1, 4):
                    kb.stt(a_.t[:], rw.t[:, k:k + TT], cw.t[:, cc, k:k + 1], a_.t[:], ALU.mult, ALU.add, [rw.r, a_.r, cw.r], [a_.r])
                kb.act(ft.t[:], a_.t[:], AF.Silu, [a_.r, cbt.r], [ft.r], bias=cbt.t[:, cc:cc + 1])
                if not isC:
                    for j in range(2):
                        kb.tr(kb.bankbf(tb)[:, j * 128:(j + 1) * 128], ft.t[:, j * 128:(j + 1) * 128], ident.t[:], [ft.r, ident.r], [kb.pr[tb]])
                    src = kb.bankbf(tb)[:, 0:256].rearrange("p (b n) -> p b n", n=128)
                    if cc < c.NCX:
                        kb.cp(sx.t[:, :, cc * 128:(cc + 1) * 128], src, [kb.pr[tb]], [sx.r], eng=("act" if cc % 2 else "dve"))
                    else:
                        g = cc - c.NCX
                        kb.cp(sb_.t[:, :, g * 128:(g + 1) * 128], src, [kb.pr[tb]], [sb_.r], eng=("act" if cc % 2 else "dve"))
                if cc >= c.NCX and ismain:
                    g = (cc - c.NCX) % 4
                    dstT = S["CT"] if isC else S["BT"]
                    m0 = t0 - (W - T)
                    kb.cp(dstT.t[:, g, m0:m0 + TT], ft.t[:], [ft.r], [dstT.rs[g]], eng="pool")
            kb.dma(io["xs_s"][t0:t0 + TT, :].rearrange("(b p) d -> p b d", p=128), sx.t[:], [sx.r], [S["xs_res"][tt]])
            kb.dma(io["B_s"][t0:t0 + TT, :].rearrange("(b p) d -> p b d", p=128), sb_.t[:], [sb_.r], [S["B_res"][tt]])
        kb.P.flush()
    with ExitStack() as st:
        sb = _sbf(nc, st)
        wdt = sb("wdt", [128, KC, c.H], F32)
        kb.dma(wdt.t[:], io["w_in"][:, c.o_dt:c.o_dt + c.H].rearrange("(kc p) n -> p kc n", p=128), [], [wdt.r])
        xf = [sb("xf%d" % i, [128, KC, 128], F32) for i in range(2)]
        dt = S["dt"]
        la = S["la"]
        for b in range(c.NW):
            x_ = xf[b % 2]
            kb.dma(x_.t[:], io["xTw"][:, 3 + b * 128:3 + (b + 1) * 128].rearrange("(kc p) n -> p kc n", p=128), [], [x_.r])
            pb = 6 + b % 2
            for kc in range(KC):
                kb.mm(kb.bank(pb, c.H), x_.t[:, kc, :], wdt.t[:, kc, :], kc == 0, kc == KC - 1, [x_.r, wdt.r], [kb.pr[pb]])
            kb.cp(dt.t[:, b, :], kb.bank(pb, c.H), [kb.pr[pb]], [dt.r], eng="act")
        vec = sb("vec", [128, 2, c.H], F32)
        kb.dma(vec.t[:, 0, :], io["dt_bias"].to_broadcast([128, c.H]), [], [vec.r])
        kb.dma(vec.t[:, 1, :], io["a_log"].to_broadcast([128, c.H]), [], [vec.r])
        vld = sb("vld", [128, c.NW], F32)
        kb.dma(vld.t[:], io["valid"], [], [vld.r])
        tmp = sb("sptmp", [128, c.NW, c.H], F32)
        NWH = [128, c.NW, c.H]
        kb.tt(dt.t[:], dt.t[:], vec.t[:, 0:1, :].to_broadcast(NWH), ALU.add, [dt.r, vec.r], [dt.r])
        kb.act(tmp.t[:], dt.t[:], AF.Abs, [dt.r], [tmp.r])
        kb.act(tmp.t[:], tmp.t[:], AF.Exp, [tmp.r], [tmp.r], scale=-1.0)
        kb.act(tmp.t[:], tmp.t[:], AF.Ln, [tmp.r], [tmp.r], bias=1.0)
        kb.ts(dt.t[:], dt.t[:], 0.0, None, ALU.max, None, [dt.r], [dt.r])
        kb.tt(dt.t[:], dt.t[:], tmp.t[:], ALU.add, [dt.r, tmp.r], [dt.r])
        kb.tt(dt.t[:], dt.t[:], vld.t[:].unsqueeze(2).to_broadcast(NWH), ALU.mult, [dt.r, vld.r], [dt.r])
        kb.act(vec.t[:, 1, :], vec.t[:, 1, :], AF.Exp, [vec.r], [vec.r])
        kb.ts(vec.t[:, 1, :], vec.t[:, 1, :], -1.0, None, ALU.mult, None, [vec.r], [vec.r])
        kb.tt(la.t[:], dt.t[:], vec.t[:, 1:2, :].to_broadcast(NWH), ALU.mult, [dt.r, vec.r], [la.r])
        kb.P.flush()


def phase2(kb, io, S):
    c = kb.c
    nc = kb.nc
    H, HG, GW, D = c.H, c.HG, c.GW, c.D
    with ExitStack() as st:
        sb = _sbf(nc, st)
        esel = sb("esel_sb", [H, H, 128], F32)
        kb.dma(esel.t[:], io["esel"].rearrange("k (h j) -> k h j", j=128), [], [esel.r])
        dsk = sb("dsk", [128, H], F32)
        kb.dma(dsk.t[:], io["d_skip"].to_broadcast([128, H]), [], [dsk.r])
        Hs = sb("Hs", [128, D], F32, 4)
        Hb = sb("Hb", [128, D], BF16, 4)
        for g in range(4):
            kb.memset(Hs.t[:, g * GW:(g + 1) * GW], 0.0, [], [Hs.rs[g]])
            kb.memset(Hb.t[:, g * GW:(g + 1) * GW], 0.0, [], [Hb.rs[g]], eng="pool")
        xs_t = [sb("xs_t%d" % i, [128, D], BF16) for i in range(2)]
        B_t = [sb("B_t%d" % i, [128, 4, 128], BF16) for i in range(2)]
        lcs = [sb("lcs%d" % i, [128, H], F32) for i in range(2)]
        sm = [sb("sm%d" % i, [128, 4, H], F32) for i in range(2)]
        xdd = [sb("xdd%d" % i, [128, D], BF16) for i in range(2)]
        xd = [sb("xd%d" % i, [128, D], BF16) for i in range(2)]
        lcT = [sb("lcT%d" % i, [H, 2, 128], F32) for i in range(2)]
        cbT = [sb("cbT%d" % i, [128, 128], F32) for i in range(2)]
        decT = [sb("decT%d" % i, [128, 4, 128], F32) for i in range(2)]
        LT = [sb("LT%d" % i, [128, 4, 128], BF16) for i in range(2)]
        t1 = [sb("t1_%d" % i, [128, GW], F32) for i in range(2)]
        t2 = [sb("t2_%d" % i, [128, GW], F32) for i in range(2)]
        tri, ones, identf, mneg = S["tri"], S["ones"], S["identf"], S["mneg"]
        dt, la, BT, CT, ytok = S["dt"], S["la"], S["BT"], S["CT"], S["ytok"]
        qi = 0
        gi = 0
        for cb in range(c.NW):
            main = cb >= c.NP
            m = cb - c.NP
            last = cb == c.NW - 1
            x_ = xs_t[cb % 2]
            kb.dma(x_.t[:], io["xs_s"][cb * 128:(cb + 1) * 128, :], S["xs_res"], [x_.r])
            x3 = x_.t[:].rearrange("p (h d) -> p h d", d=64)
            b_ = B_t[cb % 2]
            if not last:
                kb.dma(b_.t[:].rearrange("p g n -> p (g n)"), io["B_s"][cb * 128:(cb + 1) * 128, :], S["B_res"], [b_.r])
            la_c = la.t[:, cb, :]
            lc = lcs[cb % 2]
            s_ = sm[cb % 2]
            kb.mm(kb.bank(0, H), tri.t[:], la_c, True, True, [tri.r, la.r], [kb.pr[0]])
            kb.mm(kb.bank(0, H, off=H), ones.t[:], la_c, True, True, [ones.r, la.r], [kb.pr[0]])
            if main:
                kb.mm(kb.ps[0:H, 128:256], la_c, tri.t[:], True, True, [tri.r, la.r], [kb.pr[0]])
            kb.cp(lc.t[:], kb.bank(0, H), [kb.pr[0]], [lc.r])
            kb.tt(s_.t[:, 0, :], kb.bank(0, H, off=H), lc.t[:], ALU.subtract, [kb.pr[0], lc.r], [s_.r])
            kb.act(s_.t[:, 0, :], s_.t[:, 0, :], AF.Exp, [s_.r], [s_.r])
            kb.act(s_.t[:, 1, :], kb.bank(0, H, off=H), AF.Exp, [kb.pr[0]], [s_.r])
            kb.tt(s_.t[:, 2, :], dt.t[:, cb, :], s_.t[:, 0, :], ALU.mult, [dt.r, s_.r], [s_.r])
            xq = xdd[cb % 2]
            if not last:
                kb.tt(xq.t[:].rearrange("p (h d) -> p h d", d=64), x3,
                      s_.t[:, 2, :].unsqueeze(2).to_broadcast([128, H, 64]), ALU.mult, [x_.r, s_.r], [xq.r])
            if main:
                lt_ = lcT[cb % 2]
                kb.cp(lt_.t[:, 0, :], kb.ps[0:H, 128:256], [kb.pr[0]], [lt_.r])
                kb.ts(lt_.t[:, 1, :], kb.ps[0:H, 128:256], -1.0, None, ALU.mult, None, [kb.pr[0]], [lt_.r])
                kb.act(s_.t[:, 3, :], lc.t[:], AF.Exp, [lc.r], [s_.r])
                xd_ = xd[cb % 2]
                kb.tt(xd_.t[:].rearrange("p (h d) -> p h d", d=64), x3,
                      dt.t[:, cb, :].unsqueeze(2).to_broadcast([128, H, 64]), ALU.mult, [x_.r, dt.r], [xd_.r])
                mb = slice(m * 128, (m + 1) * 128)
                for g in range(4):
                    cb_ = cbT[gi % 2]
                    yb = 4 + gi % 2
                    ob = 6 + gi % 2
                    ta = t1[gi % 2]
                    tb_ = t2[gi % 2]
                    gi += 1
                    kb.mm(kb.bank(1, 128), BT.t[:, g, mb], CT.t[:, g, mb], True, True, [BT.rs[g], CT.rs[g]], [kb.pr[1]])
                    kb.cp(cb_.t[:], kb.bank(1, 128), [kb.pr[1]], [cb_.r], eng="act")
                    for q0 in range(0, HG, 4):
                        nq = min(4, HG - q0)
                        pb = 2 + qi % 2
                        dc_ = decT[qi % 2]
                        l_ = LT[qi % 2]
                        qi += 1
                        for j in range(nq):
                            h = g * HG + q0 + j
                            o_ = kb.bank(pb, 128, off=j * 128)
                            kb.mm(o_, esel.t[:, h, :], lt_.t[:, 0, :], True, False, [esel.r, lt_.r], [kb.pr[pb]])
                            kb.mm(o_, lt_.t[:, 1, :], esel.t[:, h, :], False, False, [esel.r, lt_.r], [kb.pr[pb]])
                            kb.mm(o_, identf.t[:], mneg.t[:], False, True, [identf.r, mneg.r], [kb.pr[pb]])
                        kb.act(dc_.t[:, 0:nq, :], kb.bank(pb, nq * 128).rearrange("p (q i) -> p q i", i=128), AF.Exp,
                               [kb.pr[pb]], [dc_.r])
                        kb.tt(l_.t[:, 0:nq, :], dc_.t[:, 0:nq, :], cb_.t[:].unsqueeze(1).to_broadcast([128, nq, 128]),
                              ALU.mult, [dc_.r, cb_.r], [l_.r])
                        for j in range(nq):
                            h = g * HG + q0 + j
                            kb.mm(kb.bank(yb, 64, off=(q0 + j) * 64), l_.t[:, j, :], xd_.t[:, h * 64:(h + 1) * 64], True, True,
                                  [l_.r, xd_.r], [kb.pr[yb]])
                    gs = slice(g * GW, (g + 1) * GW)
                    hs = slice(g * HG, (g + 1) * HG)
                    kb.mm(kb.bank(ob, GW), CT.t[:, g, mb], Hb.t[:, gs], True, True, [CT.rs[g], Hb.rs[g]], [kb.pr[ob]])
                    kb.tt(ta.t[:].rearrange("p (h d) -> p h d", d=64), kb.bank(ob, GW).rearrange("p (h d) -> p h d", d=64),
                          s_.t[:, 3, hs].unsqueeze(2).to_broadcast([128, HG, 64]), ALU.mult, [kb.pr[ob], s_.r], [ta.r])
                    kb.tt(ta.t[:], ta.t[:], kb.bank(yb, GW), ALU.add, [ta.r, kb.pr[yb]], [ta.r])
                    kb.tt(tb_.t[:].rearrange("p (h d) -> p h d", d=64), x3[:, hs, :],
                          dsk.t[:, hs].unsqueeze(2).to_broadcast([128, HG, 64]), ALU.mult, [x_.r, dsk.r], [tb_.r], eng="pool")
                    kb.tt(ytok.t[:, m, gs], ta.t[:], tb_.t[:], ALU.add, [ta.r, tb_.r], [ytok.rs[m]])
            if not last:
                for g in range(4):
                    gs = slice(g * GW, (g + 1) * GW)
                    hs = slice(g * HG, (g + 1) * HG)
                    sbk = 6 + gi % 2
                    gi += 1
                    kb.mm(kb.bank(sbk, GW), b_.t[:, g, :], xq.t[:, gs], True, True, [b_.r, xq.r], [kb.pr[sbk]])
                    hv = Hs.t[:, gs].rearrange("p (h d) -> p h d", d=64)
                    kb.tt(hv, hv, s_.t[:, 1, hs].unsqueeze(2).to_broadcast([128, HG, 64]), ALU.mult, [Hs.rs[g], s_.r], [Hs.rs[g]])
                    kb.tt(Hs.t[:, gs], Hs.t[:, gs], kb.bank(sbk, GW), ALU.add, [Hs.rs[g], kb.pr[sbk]], [Hs.rs[g]])
                    if cb >= c.NP - 1:
                        kb.cp(Hb.t[:, gs], Hs.t[:, gs], [Hs.rs[g]], [Hb.rs[g]], eng="act")
        kb.P.flush()


def phase3(kb, io, S):
    c = kb.c
    nc = kb.nc
    GW, D = c.GW, c.D
    with ExitStack() as st:
        sb = _sbf(nc, st)
        wz = [sb("wz%d" % i, [128, c.KC, GW], BF16) for i in range(2)]
        ng = sb("ng", [128, D], F32)
        kb.dma(ng.t[:], io["ssd_norm_g"].to_broadcast([128, D]), [], [ng.r])
        sz = [sb("sz%d" % i, [128, GW], F32) for i in range(2)]
        jk = sb("jk", [128, GW], F32)
        ss = [sb("ss%d" % i, [128, 2], F32) for i in range(2)]
        yn = [sb("yn%d" % i, [128, GW], BF16) for i in range(2)]
        ytok, ident = S["ytok"], S["ident"]
        xm = load_xm(kb, io, sb)
        ynT = sb("ynT", [128, c.KC, c.T], BF16)
        NT = GW // 128
        it = 0
        for g in range(4):
            w = wz[g % 2]
            kb.dma(w.t[:], io["w_in"][:, c.o_z + g * GW:c.o_z + (g + 1) * GW].rearrange("(kc p) n -> p kc n", p=128),
                   [], [w.r], eng="pool")
            for b in range(c.NB):
                pb = it % 4
                z_ = sz[it % 2]
                s_ = ss[it % 2]
                y_ = yn[it % 2]
                tb = 4 + it % 2
                it += 1
                tok = slice(128 + b * 128, 128 + (b + 1) * 128)
                for kc in range(c.KC):
                    kb.mm(kb.bank(pb, GW), xm.t[:, kc, tok], w.t[:, kc, :], kc == 0, kc == c.KC - 1, [xm.rs[kc], w.r], [kb.pr[pb]])
                kb.act(z_.t[:], kb.bank(pb, GW), AF.Silu, [kb.pr[pb]], [z_.r])
                kb.tt(z_.t[:], z_.t[:], ytok.t[:, b, g * GW:(g + 1) * GW], ALU.mult, [z_.r, ytok.rs[b]], [z_.r])
                kb.memset(s_.t[:], 0.0, [], [s_.r])
                kb.act(jk.t[:], z_.t[:], AF.Square, [z_.r, s_.r], [jk.r, s_.r], accum=s_.t[:, 0:1])
                kb.ts(s_.t[:, 1:2], s_.t[:, 0:1], 1.0 / GW, c.RMS_EPS, ALU.mult, ALU.add, [s_.r], [s_.r])
                kb.act(s_.t[:, 1:2], s_.t[:, 1:2], AF.Sqrt, [s_.r], [s_.r])
                kb.P.op("dve", (lambda o, i: (lambda e: e.reciprocal(out=o, in_=i)))(s_.t[:, 1:2], s_.t[:, 1:2]), [s_.r], [s_.r])
                kb.stt(y_.t[:], z_.t[:], s_.t[:, 1:2], ng.t[:, g * GW:(g + 1) * GW], ALU.mult, ALU.mult, [z_.r, s_.r, ng.r], [y_.r])
                for j in range(NT):
                    kb.tr(kb.bankbf(tb)[:, j * 128:(j + 1) * 128], y_.t[:, j * 128:(j + 1) * 128], ident.t[:],
                          [y_.r, ident.r], [kb.pr[tb]])
                kb.cp(ynT.t[:, g * NT:(g + 1) * NT, b * 128:(b + 1) * 128],
                      kb.bankbf(tb)[:, 0:NT * 128].rearrange("p (j t) -> p j t", t=128), [kb.pr[tb]], [ynT.r])
        kb.dma(io["ynT_s"], ynT.t[:], [ynT.r], [Res("ynTs")])
        kb.P.flush()


def phase4(kb, io, S):
    c = kb.c
    nc = kb.nc
    T, NB, HQ, HKV, GRP = c.T, c.NB, c.HQ, c.HKV, c.GRP
    with ExitStack() as st:
        sb = _sbf(nc, st)
        ident = S["ident"]
        xm = load_xm(kb, io, sb)
        oswaT = sb("oswaT", [128, c.QD // 128, T], BF16)
        qT = sb("qT", [64, HQ, T], BF16, HQ)
        kT = sb("kT", [64, HKV, T + 128], BF16, HKV)
        vt = sb("vt", [128, NB + 1, c.KVD], BF16)
        wq = [sb("wq%d" % i, [128, c.KC, 128], BF16) for i in range(2)]
        wv = sb("wv", [128, c.KC, c.KVD], BF16)
        bi = 0
        for pr_ in range((HQ + HKV) // 2):
            isq = pr_ < HQ // 2
            col0 = (c.o_q + pr_ * 128) if isq else (c.o_k + (pr_ - HQ // 2) * 128)
            w = wq[pr_ % 2]
            kb.dma(w.t[:], io["w_in"][:, col0:col0 + 128].rearrange("(kc p) n -> p kc n", p=128), [], [w.r], eng="pool")
            for half in range(2):
                hh = (pr_ * 2 + half) if isq else ((pr_ - HQ // 2) * 2 + half)
                t0 = 128 if isq else 0
                while t0 < T + 128:
                    n = min(512, T + 128 - t0)
                    b = bi % 4
                    bi += 1
                    for kc in range(c.KC):
                        kb.mm(kb.ps[0:64, b * 512:b * 512 + n], w.t[:, kc, half * 64:(half + 1) * 64], xm.t[:, kc, t0:t0 + n],
                              kc == 0, kc == c.KC - 1, [w.r, xm.rs[kc]], [kb.pr[b]])
                    if isq:
                        kb.act(qT.t[:, hh, t0 - 128:t0 - 128 + n], kb.ps[0:64, b * 512:b * 512 + n], AF.Copy,
                               [kb.pr[b]], [qT.rs[hh]], scale=0.125)
                    else:
                        kb.cp(kT.t[:, hh, t0:t0 + n], kb.ps[0:64, b * 512:b * 512 + n], [kb.pr[b]], [kT.rs[hh]])
                    t0 += n
        kb.dma(wv.t[:], io["w_in"][:, c.o_v:c.o_v + c.KVD].rearrange("(kc p) n -> p kc n", p=128), [], [wv.r], eng="pool")
        for b in range(NB + 1):
            pb = bi % 4
            bi += 1
            for kc in range(c.KC):
                kb.mm(kb.bank(pb, c.KVD), xm.t[:, kc, b * 128:(b + 1) * 128], wv.t[:, kc, :], kc == 0, kc == c.KC - 1,
                      [xm.rs[kc], wv.r], [kb.pr[pb]])
            kb.cp(vt.t[:, b, :], kb.bank(pb, c.KVD), [kb.pr[pb]], [vt.r], eng="act")
        bm = sb("bm", [128, 2, HQ, 256], F32)
        msk = sb("msk", [128, 2, 256], F32)
        kb.dma(bm.t[:, 0, :, :], io["swa_bias"].rearrange("h p j -> p h j"), [], [bm.r])
        kb.dma(bm.t[:, 1, :, :], io["swa_bias"].rearrange("h p j -> p h j"), [], [bm.r])
        kb.dma(msk.t[:], io["swa_mask"].rearrange("m p j -> p m j"), [], [msk.r])
        for v in range(2):
            kb.tt(bm.t[:, v, :, :], bm.t[:, v, :, :], msk.t[:, v:v + 1, :].to_broadcast([128, HQ, 256]), ALU.add,
                  [bm.r, msk.r], [bm.r])
        snk = sb("snk", [128, HQ], F32)
        kb.dma(snk.t[:], io["swa_sinks"].to_broadcast([128, HQ]), [], [snk.r])
        Sm = [sb("Sm%d" % i, [128, GRP, 256], F32) for i in range(2)]
        Pm = [sb("Pm%d" % i, [128, GRP, 256], BF16) for i in range(2)]
        PT = [sb("PT%d" % i, [128, GRP * 2, 128], BF16) for i in range(2)]
        st_ = [sb("st%d" % i, [128, 6, GRP], F32) for i in range(2)]
        rden = sb("rden", [128, HQ], F32)
        osb = [sb("osb%d" % i, [128, c.QD], BF16) for i in range(2)]
        it = 0
        NQ = c.QD // 128
        for b in range(NB):
            var = 0 if b == 0 else 1
            for hk in range(HKV):
                s2 = (it % 2) * 2
                sm_ = Sm[it % 2]
                pm_ = Pm[it % 2]
                pt_ = PT[it % 2]
                s_ = st_[it % 2]
                tbk = 4 + it % 2
                it += 1
                hsl = slice(hk * GRP, (hk + 1) * GRP)
                for g in range(GRP):
                    h = hk * GRP + g
                    col = s2 * 512 + g * 256
                    kb.mm(kb.ps[:, col:col + 256], qT.t[:, h, b * 128:(b + 1) * 128], kT.t[:, hk, b * 128:b * 128 + 256],
                          True, True, [qT.rs[h], kT.rs[hk]], [kb.pr[s2 + (g * 256) // 512]])
                nbk = (GRP * 256 + 511) // 512
                prs = [kb.pr[s2 + i] for i in range(nbk)]
                kb.tt(sm_.t[:], kb.ps[:, s2 * 512:s2 * 512 + GRP * 256].rearrange("p (g j) -> p g j", j=256),
                      bm.t[:, var, hsl, :], ALU.add, prs + [bm.r], [sm_.r])
                kb.red(s_.t[:, 0, :], sm_.t[:], ALU.max, [sm_.r], [s_.r])
                kb.tt(s_.t[:, 0, :], s_.t[:, 0, :], snk.t[:, hsl], ALU.max, [s_.r, snk.r], [s_.r])
                kb.ts(s_.t[:, 1, :], s_.t[:, 0, :], -1.0, None, ALU.mult, None, [s_.r], [s_.r])
                kb.memset(s_.t[:, 2, :], 0.0, [], [s_.r])
                for g in range(GRP):
                    kb.act(pm_.t[:, g, :], sm_.t[:, g, :], AF.Exp, [sm_.r, s_.r], [pm_.r, s_.r],
                           bias=s_.t[:, 1, g:g + 1], accum=s_.t[:, 2, g:g + 1])
                kb.tt(s_.t[:, 3, :], snk.t[:, hsl], s_.t[:, 0, :], ALU.subtract, [s_.r, snk.r], [s_.r])
                kb.act(s_.t[:, 3, :], s_.t[:, 3, :], AF.Exp, [s_.r], [s_.r])
                kb.tt(s_.t[:, 4, :], s_.t[:, 2, :], s_.t[:, 3, :], ALU.add, [s_.r], [s_.r])
                kb.P.op("dve", (lambda o, i: (lambda e: e.reciprocal(out=o, in_=i)))(rden.t[:, hsl], s_.t[:, 4, :]),
                        [s_.r], [rden.r])
                for g in range(GRP):
                    for jb in range(2):
                        kb.tr(kb.bankbf(tbk)[:, (g * 2 + jb) * 128:(g * 2 + jb + 1) * 128], pm_.t[:, g, jb * 128:(jb + 1) * 128],
                              ident.t[:], [pm_.r, ident.r], [kb.pr[tbk]])
                kb.cp(pt_.t[:], kb.bankbf(tbk)[:, 0:GRP * 256].rearrange("p (q i) -> p q i", i=128), [kb.pr[tbk]], [pt_.r],
                      eng="act")
                for g in range(GRP):
                    h = hk * GRP + g
                    ob = 6 + (h * 64) // 512
                    for jb in range(2):
                        kb.mm(kb.ps[:, 6 * 512 + h * 64:6 * 512 + (h + 1) * 64], pt_.t[:, g * 2 + jb, :],
                              vt.t[:, b + jb, hk * 64:(hk + 1) * 64], jb == 0, jb == 1, [pt_.r, vt.r], [kb.pr[ob]])
            o_ = osb[b % 2]
            nob = (c.QD + 511) // 512
            kb.tt(o_.t[:].rearrange("p (h d) -> p h d", d=64), kb.ps[:, 6 * 512:6 * 512 + c.QD].rearrange("p (h d) -> p h d", d=64),
                  rden.t[:].unsqueeze(2).to_broadcast([128, HQ, 64]), ALU.mult, [kb.pr[6 + i] for i in range(nob)] + [rden.r], [o_.r])
            tbk = 4 + it % 2
            for j in range(NQ):
                kb.tr(kb.bankbf(tbk)[:, j * 128:(j + 1) * 128], o_.t[:, j * 128:(j + 1) * 128], ident.t[:], [o_.r, ident.r], [kb.pr[tbk]])
            kb.cp(oswaT.t[:, :, b * 128:(b + 1) * 128], kb.bankbf(tbk)[:, 0:NQ * 128].rearrange("p (j t) -> p j t", t=128),
                  [kb.pr[tbk]], [oswaT.r], eng="act")
        kb.dma(io["oswaT_s"], oswaT.t[:], [oswaT.r], [Res("oswaTs")])
        kb.P.flush()


def phase5(kb, io, S):
    c = kb.c
    nc = kb.nc
    T, NB, XAH, XAD, XDC, XD, MEM = c.T, c.NB, c.XAH, c.XAD, c.XDC, c.XD, c.MEM
    NXC = XD // 128
    NMB = MEM // 128
    with ExitStack() as st:
        sb = _sbf(nc, st)
        ident = S["ident"]
        xm = load_xm(kb, io, sb)
        oxaT = sb("oxaT", [128, XD // 128, T], BF16)
        mT = sb("mT", [128, c.KC, MEM], BF16)
        kb.dma(mT.t[:], io["memT"].rearrange("(kc p) m -> p kc m", p=128), [], [mT.r], eng="pool")
        mkT = sb("mkT", [128, NXC, MEM], BF16)
        mv = sb("mv", [128, NMB, XD], BF16)
        qx = sb("qx", [128, NXC, T], BF16, NXC)
        wc = [sb("wc%d" % i, [128, c.KC, 128], BF16) for i in range(2)]
        bi = 0
        for j in range(NXC):
            w = wc[bi % 2]
            kb.dma(w.t[:], io["w_mem_kv"][:, j * 128:(j + 1) * 128].rearrange("(kc p) n -> p kc n", p=128), [], [w.r], eng="pool")
            b = bi % 4
            bi += 1
            for kc in range(c.KC):
                kb.mm(kb.bank(b, MEM), w.t[:, kc, :], mT.t[:, kc, :], kc == 0, kc == c.KC - 1, [w.r, mT.r], [kb.pr[b]])
            kb.cp(mkT.t[:, j, :], kb.bank(b, MEM), [kb.pr[b]], [mkT.r], eng="act")
        wm = [sb("wm%d" % i, [128, c.KC, 512], BF16) for i in range(2)]
        ci = 0
        for c0 in range(0, XD, 512):
            n = min(512, XD - c0)
            w = wm[ci % 2]
            ci += 1
            kb.dma(w.t[:, :, 0:n], io["w_mem_kv"][:, XD + c0:XD + c0 + n].rearrange("(kc p) n -> p kc n", p=128), [], [w.r], eng="pool")
            for mb in range(NMB):
                b = bi % 4
                bi += 1
                for kc in range(c.KC):
                    kb.mm(kb.bank(b, n), mT.t[:, kc, mb * 128:(mb + 1) * 128], w.t[:, kc, 0:n], kc == 0, kc == c.KC - 1,
                          [w.r, mT.r], [kb.pr[b]])
                kb.cp(mv.t[:, mb, c0:c0 + n], kb.bank(b, n), [kb.pr[b]], [mv.r])
        scale = float(XAD) ** -0.5
        for j in range(NXC):
            w = wc[bi % 2]
            kb.dma(w.t[:], io["w_in"][:, c.o_qx + j * 128:c.o_qx + (j + 1) * 128].rearrange("(kc p) n -> p kc n", p=128),
                   [], [w.r], eng="pool")
            t0 = 0
            while t0 < T:
                n = min(512, T - t0)
                b = bi % 4
                bi += 1
                for kc in range(c.KC):
                    kb.mm(kb.bank(b, n), w.t[:, kc, :], xm.t[:, kc, 128 + t0:128 + t0 + n], kc == 0, kc == c.KC - 1,
                          [w.r, xm.rs[kc]], [kb.pr[b]])
                kb.act(qx.t[:, j, t0:t0 + n], kb.bank(b, n), AF.Copy, [kb.pr[b]], [qx.rs[j]], scale=scale)
                t0 += n
        Sx = [sb("Sx%d" % i, [128, MEM], F32) for i in range(2)]
        Px = [sb("Px%d" % i, [128, MEM], BF16) for i in range(2)]
        PTx = [sb("PTx%d" % i, [128, NMB, 128], BF16) for i in range(2)]
        sx = [sb("sx%d" % i, [128, 4], F32) for i in range(2)]
        rdx = sb("rdx", [128, XAH], F32)
        ox = [sb("ox%d" % i, [128, XD], BF16) for i in range(2)]
        it = 0
        nob = (XD + 511) // 512
        for b in range(NB):
            for h in range(XAH):
                pb = it % 2
                s_ = sx[it % 2]
                p_ = Px[it % 2]
                pt_ = PTx[it % 2]
                tbk = 4 + it % 2
                it += 1
                for dc in range(XDC):
                    kb.mm(kb.bank(pb, MEM), qx.t[:, h * XDC + dc, b * 128:(b + 1) * 128], mkT.t[:, h * XDC + dc, :],
                          dc == 0, dc == XDC - 1, [qx.rs[h * XDC + dc], mkT.r], [kb.pr[pb]])
                kb.red(s_.t[:, 0:1], kb.bank(pb, MEM), ALU.max, [kb.pr[pb]], [s_.r])
                kb.ts(s_.t[:, 1:2], s_.t[:, 0:1], -1.0, None, ALU.mult, None, [s_.r], [s_.r])
                kb.memset(s_.t[:, 2:3], 0.0, [], [s_.r])
                kb.act(p_.t[:], kb.bank(pb, MEM), AF.Exp, [kb.pr[pb], s_.r], [p_.r, s_.r], bias=s_.t[:, 1:2], accum=s_.t[:, 2:3])
                kb.P.op("dve", (lambda o, i: (lambda e: e.reciprocal(out=o, in_=i)))(rdx.t[:, h:h + 1], s_.t[:, 2:3]),
                        [s_.r], [rdx.r])
                for mb in range(NMB):
                    kb.tr(kb.bankbf(tbk)[:, mb * 128:(mb + 1) * 128], p_.t[:, mb * 128:(mb + 1) * 128], ident.t[:],
                          [p_.r, ident.r], [kb.pr[tbk]])
                kb.cp(pt_.t[:], kb.bankbf(tbk)[:, 0:NMB * 128].rearrange("p (q i) -> p q i", i=128), [kb.pr[tbk]], [pt_.r])
                ob = 6 + (h * XAD) // 512
                for mb in range(NMB):
                    kb.mm(kb.ps[:, 6 * 512 + h * XAD:6 * 512 + (h + 1) * XAD], pt_.t[:, mb, :], mv.t[:, mb, h * XAD:(h + 1) * XAD],
                          mb == 0, mb == NMB - 1, [pt_.r, mv.r], [kb.pr[ob]])
            o_ = ox[b % 2]
            kb.tt(o_.t[:].rearrange("p (h d) -> p h d", d=XAD), kb.ps[:, 6 * 512:6 * 512 + XD].rearrange("p (h d) -> p h d", d=XAD),
                  rdx.t[:].unsqueeze(2).to_broadcast([128, XAH, XAD]), ALU.mult, [kb.pr[6 + i] for i in range(nob)] + [rdx.r], [o_.r])
            tbk = 4 + it % 2
            for j in range(NXC):
                kb.tr(kb.bankbf(tbk)[:, j * 128:(j + 1) * 128], o_.t[:, j * 128:(j + 1) * 128], ident.t[:], [o_.r, ident.r], [kb.pr[tbk]])
            kb.cp(oxaT.t[:, :, b * 128:(b + 1) * 128], kb.bankbf(tbk)[:, 0:NXC * 128].rearrange("p (j t) -> p j t", t=128),
                  [kb.pr[tbk]], [oxaT.r], eng="act")
        kb.dma(io["oxaT_s"], oxaT.t[:], [oxaT.r], [Res("oxaTs")])
        kb.P.flush()


def phase6(kb, io, S):
    c = kb.c
    nc = kb.nc
    T, D, KC = c.T, c.D, c.KC
    NQ = c.QD // 128
    NX = c.XD // 128
    with ExitStack() as st:
        sb = _sbf(nc, st)
        xm = load_xm(kb, io, sb)
        ynT = sb("ynT6", [128, KC, T], BF16)
        oswaT = sb("oswaT6", [128, NQ, T], BF16)
        oxaT = sb("oxaT6", [128, NX, T], BF16)
        mg = sb("mgT6", [128, KC, T], BF16)
        kb.dma(ynT.t[:], io["ynT_s"], [], [ynT.r])
        kb.dma(oswaT.t[:], io["oswaT_s"], [], [oswaT.r])
        kb.dma(oxaT.t[:], io["oxaT_s"], [], [oxaT.r])
        wso = [sb("wso%d" % i, [128, KC, 128], BF16) for i in range(2)]
        wwo = [sb("wwo%d" % i, [128, NQ, 128], BF16) for i in range(2)]
        wxo = [sb("wxo%d" % i, [128, NX, 128], BF16) for i in range(2)]
        wg = [sb("wg%d" % i, [128, 3, KC, 128], BF16, 3) for i in range(2)]
        sg = [sb("sg%d" % i, [128, 3, 512], F32) for i in range(2)]
        ma = [sb("ma%d" % i, [128, 512], F32) for i in range(2)]
        mb_ = [sb("mb%d" % i, [128, 512], F32) for i in range(2)]
        it = 0
        for dc in range(D // 128):
            cs = slice(dc * 128, (dc + 1) * 128)
            a, b_, x_, g_ = wso[dc % 2], wwo[dc % 2], wxo[dc % 2], wg[dc % 2]
            kb.dma(a.t[:], io["w_ssd_o"][:, cs].rearrange("(kc p) n -> p kc n", p=128), [], [a.r], eng="pool")
            kb.dma(b_.t[:], io["w_swa_o"][:, cs].rearrange("(kc p) n -> p kc n", p=128), [], [b_.r], eng="pool")
            kb.dma(x_.t[:], io["w_xa_o"][:, cs].rearrange("(kc p) n -> p kc n", p=128), [], [x_.r], eng="pool")
            for i in range(3):
                col = c.o_g + i * D + dc * 128
                kb.dma(g_.t[:, i, :, :], io["w_in"][:, col:col + 128].rearrange("(kc p) n -> p kc n", p=128), [], [g_.rs[i]], eng="pool")
            t0 = 0
            while t0 < T:
                n = min(512, T - t0)
                ts_ = slice(t0, t0 + n)
                s_ = sg[it % 2]
                m1 = ma[it % 2]
                m2 = mb_[it % 2]
                it += 1
                for kc in range(KC):
                    kb.mm(kb.bank(0, n), a.t[:, kc, :], ynT.t[:, kc, ts_], kc == 0, kc == KC - 1, [a.r, ynT.r], [kb.pr[0]])
                for kc in range(NQ):
                    kb.mm(kb.bank(1, n), b_.t[:, kc, :], oswaT.t[:, kc, ts_], kc == 0, kc == NQ - 1, [b_.r, oswaT.r], [kb.pr[1]])
                for kc in range(NX):
                    kb.mm(kb.bank(2, n), x_.t[:, kc, :], oxaT.t[:, kc, ts_], kc == 0, kc == NX - 1, [x_.r, oxaT.r], [kb.pr[2]])
                for i in range(3):
                    for kc in range(KC):
                        kb.mm(kb.bank(3 + i, n), g_.t[:, i, kc, :], xm.t[:, kc, 128 + t0:128 + t0 + n], kc == 0, kc == KC - 1,
                              [g_.rs[i], xm.rs[kc]], [kb.pr[3 + i]])
                    kb.act(s_.t[:, i, 0:n], kb.bank(3 + i, n), AF.Sigmoid, [kb.pr[3 + i]], [s_.r])
                kb.tt(m1.t[:, 0:n], s_.t[:, 0, 0:n], kb.bank(0, n), ALU.mult, [s_.r, kb.pr[0]], [m1.r])
                kb.tt(m2.t[:, 0:n], s_.t[:, 1, 0:n], kb.bank(1, n), ALU.mult, [s_.r, kb.pr[1]], [m2.r])
                kb.tt(m1.t[:, 0:n], m1.t[:, 0:n], m2.t[:, 0:n], ALU.add, [m1.r, m2.r], [m1.r])
                kb.tt(m2.t[:, 0:n], s_.t[:, 2, 0:n], kb.bank(2, n), ALU.mult, [s_.r, kb.pr[2]], [m2.r])
                kb.tt(mg.t[:, dc, ts_], m1.t[:, 0:n], m2.t[:, 0:n], ALU.add, [m1.r, m2.r], [mg.r])
                t0 += n
        kb.dma(io["mgT_s"], mg.t[:], [mg.r], [Res("mgTs")])
        kb.P.flush()


def _layernorm(kb, sb_small, x_ap, xr, out_ap, outr, gb, D, eps, jk):
    s_ = sb_small
    kb.red(s_.t[:, 0:1], x_ap, ALU.add, [xr], [s_.r])
    kb.ts(s_.t[:, 1:2], s_.t[:, 0:1], -1.0 / D, None, ALU.mult, None, [s_.r], [s_.r])
    kb.ts(x_ap, x_ap, s_.t[:, 1:2], None, ALU.add, None, [xr, s_.r], [xr])
    kb.memset(s_.t[:, 2:3], 0.0, [], [s_.r])
    kb.act(jk.t[:], x_ap, AF.Square, [xr, s_.r], [jk.r, s_.r], accum=s_.t[:, 2:3])
    kb.ts(s_.t[:, 3:4], s_.t[:, 2:3], 1.0 / D, eps, ALU.mult, ALU.add, [s_.r], [s_.r])
    kb.act(s_.t[:, 3:4], s_.t[:, 3:4], AF.Sqrt, [s_.r], [s_.r])
    kb.P.op("dve", (lambda o, i: (lambda e: e.reciprocal(out=o, in_=i)))(s_.t[:, 3:4], s_.t[:, 3:4]), [s_.r], [s_.r])
    kb.stt(x_ap, x_ap, s_.t[:, 3:4], gb.t[:, 0, :], ALU.mult, ALU.mult, [xr, s_.r, gb.r], [xr])
    kb.tt(out_ap, x_ap, gb.t[:, 1, :], ALU.add, [xr, gb.r], [outr] if outr is not xr else [xr])


def phase7(kb, io, S):
    c = kb.c
    nc = kb.nc
    T, D, KC, NB, E, CAP = c.T, c.D, c.KC, c.NB, c.E, c.CAP
    with ExitStack() as st:
        sb = _sbf(nc, st)
        acc, ident, identf = S["acc"], S["ident"], S["identf"]
        tris, ones, iota = S["tris"], S["ones"], S["iota"]
        dint, wk = S["dint"], S["wk"]
        mg = sb("mgT7", [128, KC, T], BF16)
        kb.dma(mg.t[:], io["mgT_s"], [], [mg.r])
        wo = [sb("wo%d" % i, [128, KC, 512], BF16) for i in range(2)]
        xt = [sb("xt%d" % i, [128, 512], F32) for i in range(2)]
        it = 0
        ci = 0
        for c0 in range(0, D, 512):
            n = min(512, D - c0)
            w = wo[ci % 2]
            ci += 1
            kb.dma(w.t[:, :, 0:n], io["w_out"][:, c0:c0 + n].rearrange("(kc p) n -> p kc n", p=128), [], [w.r], eng="pool")
            for b in range(NB):
                pb = it % 4
                x_ = xt[it % 2]
                it += 1
                kb.dma(x_.t[:, 0:n], io["xtok"][b * 128:(b + 1) * 128, c0:c0 + n], [], [x_.r])
                for kc in range(KC):
                    kb.mm(kb.bank(pb, n), mg.t[:, kc, b * 128:(b + 1) * 128], w.t[:, kc, 0:n], kc == 0, kc == KC - 1,
                          [mg.r, w.r], [kb.pr[pb]])
                kb.stt(acc.t[:, b, c0:c0 + n], x_.t[:, 0:n], c.ALPHA, kb.bank(pb, n), ALU.mult, ALU.add,
                       [x_.r, kb.pr[pb]], [acc.rs[b]])
        kb.P.flush()
    import os
    K7 = int(os.environ.get("K7", "9"))
    if K7 < 2:
        return
    with ExitStack() as st:
        sb = _sbf(nc, st)
        gb = sb("gb1", [128, 2, D], F32)
        kb.dma(gb.t[:, 0, :], io["ln1_g"].to_broadcast([128, D]), [], [gb.r])
        kb.dma(gb.t[:, 1, :], io["ln1_b"].to_broadcast([128, D]), [], [gb.r])
        hTb = [sb("hTb%d" % i, [128, KC, 128], BF16) for i in range(2)]
        sml = [sb("sml%d" % i, [128, 4], F32) for i in range(2)]
        jk = sb("jk7", [128, D], F32)
        hbf = [sb("hbf%d" % i, [128, D], BF16) for i in range(2)]
        lbf = [sb("lbf%d" % i, [128, D], BF16) for i in range(2)]
        lTb = [sb("lTb%d" % i, [128, KC, 128], BF16) for i in range(2)]
        rw = sb("rw", [128, KC, E], F32)
        kb.dma(rw.t[:], io["router_w"].rearrange("(kc p) e -> p kc e", p=128), [], [rw.r])
        rwh = sb("rwh", [128, KC, E], BF16)
        rwl = sb("rwl", [128, KC, E], BF16)
        kb.cp(rwh.t[:], rw.t[:], [rw.r], [rwh.r], eng="act")
        kb.tt(rwl.t[:], rw.t[:], rwh.t[:], ALU.subtract, [rw.r, rwh.r], [rwl.r])
        rbb = sb("rbb", [128, E], F32)
        kb.dma(rbb.t[:], io["router_bias"].to_broadcast([128, E]), [], [rbb.r])
        eoff = sb("eoff", [128, E], F32)
        kb.ts(eoff.t[:], iota.t[:], float(CAP), None, ALU.mult, None, [iota.r], [eoff.r])
        base = sb("base", [128, E], F32)
        kb.memset(base.t[:], 0.0, [], [base.r])
        R = [sb("rt%d" % i, [128, 8, E], F32) for i in range(2)]
        r8 = [sb("r8_%d" % i, [128, 8, 8], F32) for i in range(2)]
        idx8 = [sb("idx8_%d" % i, [128, 8], U32) for i in range(2)]
        dself = [sb("dself%d" % i, [128, 8], F32) for i in range(2)]
        for b in range(NB):
            ab = acc.t[:, b, :]
            ar = acc.rs[b]
            _layernorm(kb, sml[b % 2], ab, ar, ab, ar, gb, D, c.LN_EPS, jk)
            hb = hbf[b % 2]
            lb = lbf[b % 2]
            kb.cp(hb.t[:], ab, [ar], [hb.r], eng="act")
            kb.tt(lb.t[:], ab, hb.t[:], ALU.subtract, [ar, hb.r], [lb.r])
            hq = hTb[b % 2]
            lq = lTb[b % 2]
            ti = 0
            for src_, dst_ in ((hb, hq), (lb, lq)):
                for k0 in range(0, KC, 8):
                    nk = min(8, KC - k0)
                    pb = ti % 2
                    ti += 1
                    for j in range(nk):
                        kb.tr(kb.bankbf(pb)[:, j * 128:(j + 1) * 128], src_.t[:, (k0 + j) * 128:(k0 + j + 1) * 128], ident.t[:],
                              [src_.r, ident.r], [kb.pr[pb]])
                    kb.cp(dst_.t[:, k0:k0 + nk, :], kb.bankbf(pb)[:, 0:nk * 128].rearrange("p (j t) -> p j t", t=128),
                          [kb.pr[pb]], [dst_.r], eng=("act" if ti % 2 else "dve"))
            kb.dma(io["h1T_s"][:, :, b * 128:(b + 1) * 128], hq.t[:], [hq.r], [Res("h1Ts")])
            n3 = 3 * KC
            i3 = 0
            for kc in range(KC):
                for (l_, r_) in ((hq, rwh), (hq, rwl), (lq, rwh)):
                    kb.mm(kb.bank(2, E), l_.t[:, kc, :], r_.t[:, kc, :], i3 == 0, i3 == n3 - 1, [l_.r, r_.r], [kb.pr[2]])
                    i3 += 1
            if K7 < 3:
                continue
            r = R[b % 2]
            rr = r.r
            q = r8[b % 2]
            sc, sel, tmp, selm, M, wn, dest, oh = (r.t[:, i, :] for i in range(8))
            v3 = lambda ap: ap.rearrange("p (g e) -> p g e", e=8)
            kb.act(sc, kb.bank(2, E), AF.Sigmoid, [kb.pr[2]], [rr])
            kb.tt(sel, sc, rbb.t[:], ALU.add, [rr, rbb.r], [rr])
            kb.red(q.t[:, 0, :], v3(sel), ALU.max, [rr], [q.r])
            kb.tt(v3(tmp), v3(sel), q.t[:, 0, :].unsqueeze(2).to_broadcast([128, 8, 8]), ALU.is_equal, [rr, q.r], [rr])
            kb.stt(tmp, tmp, -1e9, sel, ALU.mult, ALU.add, [rr], [rr])
            kb.red(q.t[:, 1, :], v3(tmp), ALU.max, [rr], [q.r])
            kb.tt(q.t[:, 2, :], q.t[:, 0, :], q.t[:, 1, :], ALU.add, [q.r], [q.r])
            kb.P.op("dve", (lambda o, i: (lambda e: e.max(out=o, in_=i)))(q.t[:, 3, :], q.t[:, 2, :]), [q.r], [q.r])
            kb.ts(q.t[:, 4, :], q.t[:, 2, :], q.t[:, 3, 3:4], None, ALU.is_ge, None, [q.r], [q.r])
            kb.ts(q.t[:, 5, :], q.t[:, 4, :], 1e9, -1e9, ALU.mult, ALU.add, [q.r], [q.r])
            kb.tt(v3(selm), v3(sel), q.t[:, 4, :].unsqueeze(2).to_broadcast([128, 8, 8]), ALU.mult, [rr, q.r], [rr])
            kb.tt(v3(selm), v3(selm), q.t[:, 5, :].unsqueeze(2).to_broadcast([128, 8, 8]), ALU.add, [rr, q.r], [rr])
            kb.P.op("dve", (lambda o, i: (lambda e: e.max(out=o, in_=i)))(q.t[:, 6, :], selm), [rr], [q.r])
            ix = idx8[b % 2]
            kb.P.op("dve", (lambda o, m_, v: (lambda e: e.max_index(out=o, in_max=m_, in_values=v)))(ix.t[:], q.t[:, 6, :], selm),
                    [rr, q.r], [ix.r])
            kb.cp(q.t[:, 7, :], ix.t[:], [ix.r], [q.r])
            kb.ts(M, selm, q.t[:, 6, 7:8], None, ALU.is_ge, None, [rr, q.r], [rr])
            kb.tt(wn, sc, M, ALU.mult, [rr], [rr])
            s_ = sml[b % 2]
            kb.red(s_.t[:, 0:1], wn, ALU.add, [rr], [s_.r])
            kb.P.op("dve", (lambda o, i: (lambda e: e.reciprocal(out=o, in_=i)))(s_.t[:, 1:2], s_.t[:, 0:1]), [s_.r], [s_.r])
            kb.ts(wn, wn, s_.t[:, 1:2], 2.5, ALU.mult, ALU.mult, [rr, s_.r], [rr])
            kb.cp(S["wn"].t[:, b, :], wn, [rr], [S["wn"].r], eng="act")
        kb.P.flush()


def phase9(kb, io, S):
    c = kb.c
    nc = kb.nc
    T, D, KC, NB, E, CAP, ED, SD = c.T, c.D, c.KC, c.NB, c.E, c.CAP, c.ED, c.SD
    NF = ED // 128
    NS = CAP // 128
    with ExitStack() as st:
        sb = _sbf(nc, st)
        acc, ident = S["acc"], S["ident"]
        h1T = sb("h1T9", [128, KC, T], BF16)
        kb.dma(h1T.t[:], io["h1T_s"], [], [h1T.r])
        for b in range(NB):
            kb.ts(acc.t[:, b, :], acc.t[:, b, :], c.ALPHA, None, ALU.mult, None, [acc.rs[b]], [acc.rs[b]])
        w1 = [sb("w1_%d" % i, [128, KC, ED], BF16) for i in range(1)]
        w3 = [sb("w3_%d" % i, [128, KC, ED], BF16) for i in range(1)]
        w2 = [sb("w2_%d" % i, [128, NF, D], BF16) for i in range(1)]
        hsT = sb("hsT", [128, SD // 128, T], BF16)
        sl = [sb("sl%d" % i, [128, 512], F32) for i in range(2)]
        a, b3, b2 = w1[0], w3[0], w2[0]
        kb.dma(a.t[:], io["ws1"].rearrange("(kc p) n -> p kc n", p=128), [], [a.r], eng="pool")
        kb.dma(b3.t[:], io["ws3"].rearrange("(kc p) n -> p kc n", p=128), [], [b3.r], eng="pool")
        kb.dma(b2.t[:], io["ws2"].rearrange("(f p) n -> p f n", p=128), [], [b2.r], eng="pool")
        it = 0
        for fc in range(SD // 128):
            t0 = 0
            while t0 < T:
                n = min(512, T - t0)
                s_ = sl[it % 2]
                p1 = (it % 2) * 2
                it += 1
                for kc in range(KC):
                    kb.mm(kb.bank(p1, n), a.t[:, kc, fc * 128:(fc + 1) * 128], h1T.t[:, kc, t0:t0 + n], kc == 0, kc == KC - 1,
                          [a.r, h1T.r], [kb.pr[p1]])
                for kc in range(KC):
                    kb.mm(kb.bank(p1 + 1, n), b3.t[:, kc, fc * 128:(fc + 1) * 128], h1T.t[:, kc, t0:t0 + n], kc == 0, kc == KC - 1,
                          [b3.r, h1T.r], [kb.pr[p1 + 1]])
                kb.act(s_.t[:, 0:n], kb.bank(p1, n), AF.Silu, [kb.pr[p1]], [s_.r])
                kb.tt(hsT.t[:, fc, t0:t0 + n], s_.t[:, 0:n], kb.bank(p1 + 1, n), ALU.mult, [s_.r, kb.pr[p1 + 1]], [hsT.r])
                t0 += n
        for b in range(NB):
            for c0 in range(0, D, 512):
                n = min(512, D - c0)
                pb = 4 + it % 4
                it += 1
                for fc in range(SD // 128):
                    kb.mm(kb.bank(pb, n), hsT.t[:, fc, b * 128:(b + 1) * 128], b2.t[:, fc, c0:c0 + n], fc == 0, fc == SD // 128 - 1,
                          [hsT.r, b2.r], [kb.pr[pb]])
                kb.tt(acc.t[:, b, c0:c0 + n], acc.t[:, b, c0:c0 + n], kb.bank(pb, n), ALU.add, [acc.rs[b], kb.pr[pb]], [acc.rs[b]])
        kb.P.flush()
    with ExitStack() as st:
        sb = _sbf(nc, st)
        w1 = [sb("w1r%d" % i, [128, KC, ED], BF16) for i in range(2)]
        w3 = [sb("w3r%d" % i, [128, KC, ED], BF16) for i in range(2)]
        w2 = [sb("w2r%d" % i, [128, NF, D], BF16, D // 512) for i in range(1)]
        sl = [sb("slr%d" % i, [128, 512], F32) for i in range(2)]
        h1T = sb("h1T9b", [128, KC, T], BF16)
        kb.dma(h1T.t[:], io["h1T_s"], [], [h1T.r])
        hT = sb("hTd", [128, NF, T], BF16)
        wn = S["wn"]
        it = 0
        def load_gu(e):
            a, b3 = w1[e % 2], w3[e % 2]
            kb.dma(a.t[:], io["w1"][e].rearrange("(kc p) n -> p kc n", p=128), [], [a.r], eng="pool")
            kb.dma(b3.t[:], io["w3"][e].rearrange("(kc p) n -> p kc n", p=128), [], [b3.r], eng="pool")

        def load_dn(e):
            b2 = w2[0]
            for ci in range(D // 512):
                kb.dma(b2.t[:, :, ci * 512:(ci + 1) * 512],
                       io["w2"][e][:, ci * 512:(ci + 1) * 512].rearrange("(f p) n -> p f n", p=128),
                       [], [b2.rs[ci]], eng="pool")

        load_gu(0)
        load_dn(0)
        load_gu(1)
        for e in range(E):
            a, b3, b2 = w1[e % 2], w3[e % 2], w2[0]
            for fc in range(NF):
                t0 = 0
                while t0 < T:
                    n = min(512, T - t0)
                    s_ = sl[it % 2]
                    p1 = (it % 2) * 2
                    it += 1
                    for kc in range(KC):
                        kb.mm(kb.bank(p1, n), a.t[:, kc, fc * 128:(fc + 1) * 128], h1T.t[:, kc, t0:t0 + n], kc == 0, kc == KC - 1,
                              [a.r, h1T.r], [kb.pr[p1]])
                    for kc in range(KC):
                        kb.mm(kb.bank(p1 + 1, n), b3.t[:, kc, fc * 128:(fc + 1) * 128], h1T.t[:, kc, t0:t0 + n], kc == 0, kc == KC - 1,
                              [b3.r, h1T.r], [kb.pr[p1 + 1]])
                    kb.act(s_.t[:, 0:n], kb.bank(p1, n), AF.Silu, [kb.pr[p1]], [s_.r])
                    kb.tt(hT.t[:, fc, t0:t0 + n], s_.t[:, 0:n], kb.bank(p1 + 1, n), ALU.mult, [s_.r, kb.pr[p1 + 1]], [hT.r])
                    t0 += n
            if e + 2 < E:
                load_gu(e + 2)
            for c0 in range(0, D, 512):
                for b in range(NB):
                    n = min(512, D - c0)
                    pb = 4 + it % 4
                    it += 1
                    for fc in range(NF):
                        kb.mm(kb.bank(pb, n), hT.t[:, fc, b * 128:(b + 1) * 128], b2.t[:, fc, c0:c0 + n], fc == 0, fc == NF - 1,
                              [hT.r, b2.rs[c0 // 512]], [kb.pr[pb]])
                    kb.stt(acc.t[:, b, c0:c0 + n], kb.bank(pb, n), wn.t[:, b, e:e + 1], acc.t[:, b, c0:c0 + n], ALU.mult, ALU.add,
                           [kb.pr[pb], wn.r, acc.rs[b]], [acc.rs[b]])
            if e + 1 < E:
                load_dn(e + 1)
        kb.P.flush()


def phase10(kb, io, S):
    c = kb.c
    nc = kb.nc
    D, NB, E, CAP = c.D, c.NB, c.E, c.CAP
    with ExitStack() as st:
        sb = _sbf(nc, st)
        acc = S["acc"]
        gb = sb("gb2", [128, 2, D], F32)
        kb.dma(gb.t[:, 0, :], io["ln2_g"].to_broadcast([128, D]), [], [gb.r])
        kb.dma(gb.t[:, 1, :], io["ln2_b"].to_broadcast([128, D]), [], [gb.r])
        NH = (D + 1023) // 1024
        sml = [sb("sml2_%d" % i, [128, 4], F32) for i in range(2)]
        jk = sb("jk10", [128, D], F32)
        for b in range(NB):
            _layernorm(kb, sml[b % 2], acc.t[:, b, :], acc.rs[b], acc.t[:, b, :], acc.rs[b], gb, D, c.LN_EPS, jk)
            kb.dma(io["out"][b * 128:(b + 1) * 128, :], acc.t[:, b, :], [acc.rs[b]], [Res("out")])
        kb.P.flush()


def build(cfg, STOP=99):
    c = cfg
    nc = bass.Bass("TRN2", target_bir_lowering=False)

    def din(name, shape, dt=F32):
        return nc.dram_tensor(name, list(shape), dt, kind="ExternalInput").ap()

    def scr(name, shape, dt):
        return nc.dram_tensor(name, list(shape), dt).ap()

    io = {}
    specs = {
        "xTw": [c.D, c.W + 3], "xtok": [c.T, c.D], "valid": [128, c.NW], "memT": [c.D, c.MEM],
        "w_in": [c.D, c.IN], "conv_wT": [128, c.NCC, 4], "conv_bT": [128, c.NCC],
        "dt_bias": [1, c.H], "a_log": [1, c.H], "d_skip": [1, c.H], "ssd_norm_g": [1, c.D],
        "swa_sinks": [1, c.HQ], "swa_bias": [c.HQ, 128, 256], "swa_mask": [2, 128, 256],
        "w_mem_kv": [c.D, 2 * c.XD], "w_ssd_o": [c.D, c.D], "w_swa_o": [c.QD, c.D], "w_xa_o": [c.XD, c.D],
        "w_out": [c.D, c.D], "ln1_g": [1, c.D], "ln1_b": [1, c.D], "router_w": [c.D, c.E], "router_bias": [1, c.E],
        "w1": [c.E, c.D, c.ED], "w3": [c.E, c.D, c.ED], "w2": [c.E, c.ED, c.D],
        "ws1": [c.D, c.SD], "ws3": [c.D, c.SD], "ws2": [c.SD, c.D], "ln2_g": [1, c.D], "ln2_b": [1, c.D],
        "cident": [128, 128], "ctri": [128, 128], "ctris": [128, 128], "cones": [128, 128], "cmneg": [128, 128],
        "ciota": [128, 64], "esel": [c.H, c.H * 128],
    }
    for k, shp in specs.items():
        io[k] = din(k, shp)
    io["out"] = nc.dram_tensor("out", [c.T, c.D], F32, kind="ExternalOutput").ap()
    io["xs_s"] = scr("xs_s", [c.W, c.D], BF16)
    io["B_s"] = scr("B_s", [c.W, 512], BF16)
    io["ynT_s"] = scr("ynT_s", [128, c.KC, c.T], BF16)
    io["oswaT_s"] = scr("oswaT_s", [128, c.QD // 128, c.T], BF16)
    io["oxaT_s"] = scr("oxaT_s", [128, c.XD // 128, c.T], BF16)
    io["mgT_s"] = scr("mgT_s", [128, c.KC, c.T], BF16)
    io["h1T_s"] = scr("h1T_s", [128, c.KC, c.T], BF16)
    NH = (c.D + 1023) // 1024
    io["Xg"] = [scr("Xg%d" % i, [c.E * c.CAP, min(1024, c.D)], BF16) for i in range(NH)]
    io["Yg"] = [scr("Yg%d" % i, [c.E * c.CAP, min(1024, c.D)], F32) for i in range(NH)]
    with ExitStack() as es:
        kb = KB(nc, c, es)
        sb = _sbf(nc, es)
        S = {}
        S["ident"] = sb("ident", [128, 128], BF16)
        kb.dma(S["ident"].t[:], io["cident"], [], [S["ident"].r], eng="pool")
        for nm, src in (("identf", "cident"), ("tri", "ctri"), ("tris", "ctris"), ("ones", "cones"), ("mneg", "cmneg")):
            S[nm] = sb(nm, [128, 128], F32)
            kb.dma(S[nm].t[:], io[src], [], [S[nm].r])
        S["iota"] = sb("iota", [128, 64], F32)
        kb.dma(S["iota"].t[:], io["ciota"], [], [S["iota"].r])
        with ExitStack() as s1:
            sb1 = _sbf(nc, s1)
            S["BT"] = sb1("BT", [128, 4, c.T], BF16, 4)
            S["CT"] = sb1("CT", [128, 4, c.T], BF16, 4)
            S["dt"] = sb1("dt", [128, c.NW, c.H], F32)
            S["la"] = sb1("la", [128, c.NW, c.H], F32)
            S["ytok"] = sb1("ytok", [128, c.NB, c.D], BF16, c.NB)
            S["xs_res"] = [Res("xs%d" % i) for i in range(c.W // 256)]
            S["B_res"] = [Res("Bs%d" % i) for i in range(c.W // 256)]
            phase1(kb, io, S)
            if STOP >= 2:
                phase2(kb, io, S)
            if STOP >= 3:
                phase3(kb, io, S)
        if STOP >= 4:
            phase4(kb, io, S)
        if STOP >= 5:
            phase5(kb, io, S)
        if STOP >= 6:
            phase6(kb, io, S)
        with ExitStack() as s2:
            if STOP < 7:
                return nc
            sb2 = _sbf(nc, s2)
            S["acc"] = sb2("accum", [128, c.NB, c.D], F32, c.NB)
            S["dint"] = sb2("dint", [128, c.NB, 8], I32, c.NB)
            S["wk"] = sb2("wk", [128, c.NB, 8], F32)
            S["wn"] = sb2("wn", [128, c.NB, c.E], F32)
            phase7(kb, io, S)
            if STOP >= 9:
                phase9(kb, io, S)
            if STOP >= 10:
                phase10(kb, io, S)
        print("bass ops:", kb.P.nops)
    return nc


def t5_bucket(dist):
    dist = np.asarray(dist)
    max_exact = 16
    d = np.maximum(dist, 1).astype(np.float32)
    large = max_exact + (np.log(d / np.float32(max_exact)) / np.float32(math.log(128 / max_exact)) * np.float32(32 - max_exact)).astype(np.int32)
    large = np.minimum(large, 31)
    return np.where(dist < max_exact, dist, large)


def host_constants(cfg):
    c = cfg
    j = np.arange(128)[:, None]
    i = np.arange(128)[None, :]
    k = {}
    k["cident"] = (j == i).astype(np.float32)
    k["ctri"] = (j <= i).astype(np.float32)
    k["ctris"] = (j < i).astype(np.float32)
    k["cones"] = np.ones((128, 128), np.float32)
    k["cmneg"] = np.where(j <= i, 0.0, NEG).astype(np.float32)
    k["ciota"] = np.tile(np.arange(64, dtype=np.float32)[None, :], (128, 1))
    es = np.zeros((c.H, c.H, 128), np.float32)
    for h in range(c.H):
        es[h, h, :] = 1.0
    k["esel"] = es.reshape(c.H, c.H * 128)
    return k


def prep_core(cfg, inp, core, consts):
    c = cfg
    b = core // c.CPS
    s0 = (core % c.CPS) * c.T
    x = inp["x"]
    m = {}
    lo = s0 + c.T - c.W - 3
    xw = np.zeros((c.W + 3, c.D), np.float32)
    src_lo = max(lo, 0)
    xw[src_lo - lo:, :] = x[b, src_lo:s0 + c.T, :]
    m["xTw"] = np.ascontiguousarray(xw.T)
    m["xtok"] = np.ascontiguousarray(x[b, s0:s0 + c.T, :])
    pos = s0 + c.T - c.W + np.arange(c.W)
    m["valid"] = np.ascontiguousarray((pos >= 0).astype(np.float32).reshape(c.NW, 128).T)
    m["memT"] = np.ascontiguousarray(inp["mem"][b].T)
    m["w_in"] = inp["w_in"][0]
    cwv = inp["conv_w"][0]
    m["conv_wT"] = np.ascontiguousarray(cwv.T.reshape(c.NCC, 128, 4).transpose(1, 0, 2))
    m["conv_bT"] = np.ascontiguousarray(inp["conv_b"][0].reshape(c.NCC, 128).T)
    for nm in ("dt_bias", "a_log", "d_skip", "ssd_norm_g", "swa_sinks", "ln1_g", "ln1_b", "router_bias", "ln2_g", "ln2_b"):
        m[nm] = np.ascontiguousarray(inp[nm][0].reshape(1, -1))
    qi = np.arange(128)[:, None]
    kj = np.arange(256)[None, :]
    dist = qi + 128 - kj
    bias = inp["rel_bias"][t5_bucket(np.maximum(dist, 0))]
    m["swa_bias"] = np.ascontiguousarray(bias.transpose(2, 0, 1))
    inwin = (dist >= 0) & (dist < 128)
    mrest = np.where(inwin, 0.0, NEG).astype(np.float32)
    mfirst = np.where(inwin & (kj >= 128), 0.0, NEG).astype(np.float32)
    m["swa_mask"] = np.stack([mfirst if s0 == 0 else mrest, mrest]).astype(np.float32)
    for nm in ("w_mem_kv", "w_ssd_o", "w_swa_o", "w_xa_o", "w_out", "router_w", "w1", "w3", "w2", "ws1", "ws3", "ws2"):
        m[nm] = inp[nm][0]
    m.update(consts)
    return m


_NC_CACHE = {}


def kernel(**inputs):
    cfg = Cfg()
    inp = {k: np.asarray(v) for k, v in inputs.items()}
    if "nc" not in _NC_CACHE:
        _NC_CACHE["nc"] = build(cfg)
    nc = _NC_CACHE["nc"]
    consts = host_constants(cfg)
    in_maps = [prep_core(cfg, inp, core, consts) for core in range(cfg.NCORES)]
    res = run_bass_kernel_spmd(nc, in_maps, core_ids=list(range(cfg.NCORES)))
    out = np.zeros((cfg.BATCH, cfg.CPS * cfg.T, cfg.D), np.float32)
    for core in range(cfg.NCORES):
        b = core // cfg.CPS
        s0 = (core % cfg.CPS) * cfg.T
        out[b, s0:s0 + cfg.T, :] = res.results[core]["out"]
    return out
```

```python
from contextlib import ExitStack
import math
import numpy as np
import concourse.bass as bass
import concourse.mybir as mybir
from concourse.bass_utils import run_bass_kernel_spmd

F32 = mybir.dt.float32
BF16 = mybir.dt.bfloat16
I32 = mybir.dt.int32
U32 = mybir.dt.uint32
AF = mybir.ActivationFunctionType
ALU = mybir.AluOpType
AX = mybir.AxisListType

ENGS = ("pe", "act", "dve", "pool", "sp")
NEG = -30000.0


class Cfg:
    def __init__(self, D=2048, T=1024, W=4096, HQ=16, HKV=4, XAH=4, MEM=256, CAP=256, NCORES=8, BATCH=2):
        self.D = D
        self.T = T
        self.W = W
        self.KC = D // 128
        self.H = D // 64
        self.G = 4
        self.HG = self.H // 4
        self.GW = D // 4
        self.N = 128
        self.NCX = D // 128
        self.NCC = self.NCX + 8
        self.CONV = D + 1024
        self.HQ = HQ
        self.HKV = HKV
        self.GRP = HQ // HKV
        self.QD = HQ * 64
        self.KVD = HKV * 64
        self.XAH = XAH
        self.XAD = D // 8
        self.XD = XAH * self.XAD
        self.XDC = self.XAD // 128
        self.MEM = MEM
        self.E = 64
        self.ED = D // 4
        self.SD = D // 4
        self.CAP = CAP
        self.NB = T // 128
        self.NW = W // 128
        self.NP = self.NW - self.NB
        sizes = (D, self.CONV, self.H, self.QD, self.KVD, self.KVD, self.XD, 3 * D)
        offs = [0]
        for s in sizes:
            offs.append(offs[-1] + s)
        (self.o_z, self.o_xbc, self.o_dt, self.o_q, self.o_k, self.o_v, self.o_qx, self.o_g, self.IN) = offs
        self.NCORES = NCORES
        self.BATCH = BATCH
        self.CPS = NCORES // BATCH
        self.ALPHA = 2.0 ** 0.25
        self.LN_EPS = 1e-5
        self.RMS_EPS = 1e-5


class Res:
    __slots__ = ("name", "w", "r")

    def __init__(self, name):
        self.name = name
        self.w = None
        self.r = {}


class Op:
    __slots__ = ("eng", "fn", "deps", "pos", "signal", "sigval", "is_dma", "dsem", "dval",
                 "waits", "flushed", "uid")


class Prog:
    def __init__(self, nc, es, n_dma_sems=24):
        self.nc = nc
        self.esem = {e: es.enter_context(nc.semaphore("s_" + e)) for e in ENGS}
        self.dsems = [es.enter_context(nc.semaphore("d%d" % i)) for i in range(n_dma_sems)]
        self.dval = [0] * n_dma_sems
        self.dlast = [None] * n_dma_sems
        self.dnext = 0
        self.dnext_sw = 0
        self.pending = {e: [] for e in ENGS}
        self.pos = {e: 0 for e in ENGS}
        self.sigcnt = {e: 0 for e in ENGS}
        self.seen = {e: {} for e in ENGS}
        self.seen_d = {e: {} for e in ENGS}
        self.uid = 0
        self.nops = 0

    def _mk(self, eng, fn, reads, writes, is_dma):
        o = Op()
        o.eng = eng
        o.fn = fn
        o.is_dma = is_dma
        o.signal = False
        o.sigval = 0
        o.flushed = False
        o.waits = None
        self.uid += 1
        o.uid = self.uid
        deps = {}
        for r in reads:
            if r.w is not None:
                deps[r.w.uid] = r.w
        for w in writes:
            if w.w is not None:
                deps[w.w.uid] = w.w
            for rd in w.r.values():
                deps[rd.uid] = rd
        o.deps = [d for d in deps.values() if not d.flushed]
        key = ("d", o.uid) if is_dma else eng
        for r in reads:
            r.r[key] = o
        for w in writes:
            w.w = o
            w.r = {}
        self.pos[eng] += 1
        o.pos = self.pos[eng]
        self.pending[eng].append(o)
        return o

    def op(self, eng, fn, reads=(), writes=()):
        return self._mk(eng, fn, reads, writes, False)

    def dma(self, eng, fn, reads=(), writes=(), n=1):
        o = self._mk(eng, fn, reads, writes, True)
        half = len(self.dsems) // 2
        if eng == "pool":
            s = half + self.dnext_sw
            self.dnext_sw = (self.dnext_sw + 1) % (len(self.dsems) - half)
        else:
            s = self.dnext
            self.dnext = (self.dnext + 1) % half
        prev = self.dlast[s]
        if prev is not None and not prev.flushed:
            o.deps.append(prev)
        self.dval[s] += 16 * n
        o.dsem = s
        o.dval = self.dval[s]
        self.dlast[s] = o
        return o

    def flush(self):
        nc = self.nc
        for e in ENGS:
            seen = self.seen[e]
            seen_d = self.seen_d[e]
            for o in self.pending[e]:
                waits = []
                for d in o.deps:
                    if d.flushed:
                        continue
                    if d.is_dma:
                        if seen_d.get(d.dsem, 0) >= d.dval:
                            continue
                        seen_d[d.dsem] = d.dval
                        waits.append(d)
                    else:
                        if d.eng == e and e == "pe":
                            continue
                        if seen.get(d.eng, 0) >= d.pos:
                            continue
                        seen[d.eng] = d.pos
                        d.signal = True
                        waits.append(d)
                o.waits = waits
        endops = {}
        for e in ENGS:
            for o in reversed(self.pending[e]):
                if not o.is_dma:
                    o.signal = True
                    endops[e] = o
                    break
        for e in ENGS:
            for o in self.pending[e]:
                if o.signal and not o.is_dma:
                    self.sigcnt[e] += 1
                    o.sigval = self.sigcnt[e]
        esem = self.esem
        dsems = self.dsems
        dfinal = [(i, self.dval[i]) for i in range(len(dsems))
                  if self.dlast[i] is not None and not self.dlast[i].flushed]
        pending = self.pending

        def emit(e, eng):
            for o in pending[e]:
                for d in o.waits:
                    if d.is_dma:
                        eng.wait_ge(dsems[d.dsem], d.dval)
                    else:
                        eng.wait_ge(esem[d.eng], d.sigval)
                if o.is_dma:
                    for ins in o.fn(eng):
                        ins.then_inc(dsems[o.dsem], 16)
                else:
                    ins = o.fn(eng)
                    if o.signal:
                        ins.then_inc(esem[e], 1)
            for f in ENGS:
                if f != e and f in endops:
                    eng.wait_ge(esem[f], endops[f].sigval)
            for si, v in dfinal:
                eng.wait_ge(dsems[si], v)

        with nc.Block() as block:
            @block.tensor
            def _(eng):
                emit("pe", eng)

            @block.scalar
            def _(eng):
                emit("act", eng)

            @block.vector
            def _(eng):
                emit("dve", eng)

            @block.gpsimd
            def _(eng):
                emit("pool", eng)

            @block.sync
            def _(eng):
                emit("sp", eng)
        for e in ENGS:
            self.nops += len(self.pending[e])
            for o in self.pending[e]:
                o.flushed = True
                o.fn = None
                o.deps = None
            self.pending[e] = []


class Tl:
    __slots__ = ("t", "r", "rs")

    def __init__(self, t, name, nres=0):
        self.t = t
        self.r = Res(name)
        self.rs = [Res(name + str(i)) for i in range(nres)]


class KB:
    def __init__(self, nc, cfg, es):
        self.nc = nc
        self.c = cfg
        self.P = Prog(nc, es)
        self.ps = es.enter_context(nc.psum_tensor("PS", [128, 4096], F32))
        self.pr = [Res("ps%d" % i) for i in range(8)]

    def bank(self, i, n=512, off=0):
        return self.ps[:, i * 512 + off:i * 512 + off + n]

    def bankbf(self, i):
        return self.ps[:, i * 512:(i + 1) * 512].bitcast(BF16)

    def mm(self, out, lhsT, rhs, start, stop, R, W):
        return self.P.op("pe", lambda e: e.matmul(out, lhsT=lhsT, rhs=rhs, start=start, stop=stop), R, W)

    def tr(self, out, in_, ident, R, W):
        return self.P.op("pe", lambda e: e.transpose(out, in_, ident), R, W)

    def act(self, out, in_, func, R, W, bias=None, scale=1.0, accum=None):
        kw = {}
        if bias is not None:
            kw["bias"] = bias
        if accum is not None:
            kw["accum_out"] = accum
        return self.P.op("act", lambda e: e.activation(out=out, in_=in_, func=func, scale=scale, **kw), R, W)

    def tt(self, out, in0, in1, op, R, W, eng="dve"):
        return self.P.op(eng, lambda e: e.tensor_tensor(out=out, in0=in0, in1=in1, op=op), R, W)

    def ts(self, out, in0, s1, s2, op0, op1, R, W, accum=None, eng="dve"):
        kw = {}
        if accum is not None:
            kw["accum_out"] = accum
        if op1 is None:
            return self.P.op(eng, lambda e: e.tensor_scalar(out=out, in0=in0, scalar1=s1, scalar2=None, op0=op0, **kw), R, W)
        return self.P.op(eng, lambda e: e.tensor_scalar(out=out, in0=in0, scalar1=s1, scalar2=s2, op0=op0, op1=op1, **kw), R, W)

    def stt(self, out, in0, scalar, in1, op0, op1, R, W, eng="dve"):
        return self.P.op(eng, lambda e: e.scalar_tensor_tensor(out=out, in0=in0, scalar=scalar, in1=in1, op0=op0, op1=op1), R, W)

    def cp(self, out, in_, R, W, eng="dve"):
        if eng == "act":
            return self.P.op("act", lambda e: e.copy(out=out, in_=in_), R, W)
        return self.P.op(eng, lambda e: e.tensor_copy(out=out, in_=in_), R, W)

    def red(self, out, in_, op, R, W, eng="dve"):
        return self.P.op(eng, lambda e: e.tensor_reduce(out=out, in_=in_, axis=AX.X, op=op), R, W)

    def memset(self, out, val, R, W, eng="dve"):
        return self.P.op(eng, lambda e: e.memset(out, val), R, W)

    def dma(self, out, in_, R, W, eng="sp"):
        return self.P.dma(eng, lambda e: [e.dma_start(out=out, in_=in_)], R, W)

    def scatter(self, out, offs, in_, bound, R, W):
        return self.P.dma("pool", lambda e: [e.indirect_dma_start(
            out=out, out_offset=bass.IndirectOffsetOnAxis(ap=offs, axis=0), in_=in_, in_offset=None,
            bounds_check=bound, oob_is_err=False)], R, W)

    def gather(self, out, in_, offs, bound, R, W):
        return self.P.dma("pool", lambda e: [e.indirect_dma_start(
            out=out, out_offset=None, in_=in_, in_offset=bass.IndirectOffsetOnAxis(ap=offs, axis=0),
            bounds_check=bound, oob_is_err=False)], R, W)


_UNIQ = [0]


def _sbf(nc, stack):
    def sb(name, shape, dt, nres=0):
        _UNIQ[0] += 1
        name = "%s_%d" % (name, _UNIQ[0])
        return Tl(stack.enter_context(nc.sbuf_tensor(name, shape, dt)), name, nres)
    return sb


def load_xm(kb, io, sb):
    c = kb.c
    xm = sb("xm", [128, c.KC, 128 + c.T], BF16, c.KC)
    lo = 3 + c.W - c.T - 128
    for kc in range(c.KC):
        kb.dma(xm.t[:, kc, :], io["xTw"][kc * 128:(kc + 1) * 128, lo:lo + 128 + c.T], [], [xm.rs[kc]], eng="pool")
    return xm


def phase1(kb, io, S):
    c = kb.c
    nc = kb.nc
    W, T, KC = c.W, c.T, c.KC
    TT = 256
    with ExitStack() as st:
        sb = _sbf(nc, st)
        wall = sb("wall", [128, KC, c.CONV], BF16, c.NCC)
        for cc in range(c.NCC):
            col0 = c.o_xbc + cc * 128
            kb.dma(wall.t[:, :, cc * 128:(cc + 1) * 128], io["w_in"][:, col0:col0 + 128].rearrange("(kc p) n -> p kc n", p=128),
                   [], [wall.rs[cc]], eng="pool")
        cw = sb("cw", [128, c.NCC, 4], F32)
        cbt = sb("cbt", [128, c.NCC], F32)
        kb.dma(cw.t[:], io["conv_wT"], [], [cw.r])
        kb.dma(cbt.t[:], io["conv_bT"], [], [cbt.r])
        xw = [sb("xw%d" % i, [128, KC, TT + 3], BF16) for i in range(2)]
        raw = [sb("raw%d" % i, [128, TT + 3], F32) for i in range(2)]
        acc = [sb("acc%d" % i, [128, TT], F32) for i in range(2)]
        feat = [sb("feat%d" % i, [128, TT], BF16) for i in range(2)]
        sgx = [sb("sgx%d" % i, [128, 2, c.D], BF16) for i in range(2)]
        sgb = [sb("sgb%d" % i, [128, 2, 512], BF16) for i in range(2)]
        ident = S["ident"]
        it = 0
        for tt in range(W // TT):
            t0 = tt * TT
            ismain = t0 >= W - T
            x_ = xw[tt % 2]
            kb.dma(x_.t[:], io["xTw"][:, t0:t0 + TT + 3].rearrange("(kc p) n -> p kc n", p=128), [], [x_.r], eng="pool")
            sx = sgx[tt % 2]
            sb_ = sgb[tt % 2]
            for cc in range(c.NCC):
                isC = cc >= c.NCX + 4
                if isC and not ismain:
                    continue
                b = it % 4
                rw = raw[it % 2]
                a_ = acc[it % 2]
                ft = feat[it % 2]
                tb = 4 + it % 4
                it += 1
                for kc in range(KC):
                    kb.mm(kb.bank(b, TT + 3), wall.t[:, kc, cc * 128:(cc + 1) * 128], x_.t[:, kc, :], kc == 0, kc == KC - 1,
                          [wall.rs[cc], x_.r], [kb.pr[b]])
                kb.cp(rw.t[:], kb.bank(b, TT + 3), [kb.pr[b]], [rw.r], eng="act")
                kb.ts(a_.t[:], rw.t[:, 0:TT], cw.t[:, cc, 0:1], None, ALU.mult, None, [rw.r, cw.r], [a_.r])
                for k in range(1, 4):
                    kb.stt(a_.t[:], rw.t[:, k:k + TT], cw.t[:, cc, k:k + 1], a_.t[:], ALU.mult, ALU.add, [rw.r, a_.r, cw.r], [a_.r])
                kb.act(ft.t[:], a_.t[:], AF.Silu, [a_.r, cbt.r], [ft.r], bias=cbt.t[:, cc:cc + 1])
                if not isC:
                    for j in range(2):
                        kb.tr(kb.bankbf(tb)[:, j * 128:(j + 1) * 128], ft.t[:, j * 128:(j + 1) * 128], ident.t[:], [ft.r, ident.r], [kb.pr[tb]])
                    src = kb.bankbf(tb)[:, 0:256].rearrange("p (b n) -> p b n", n=128)
                    if cc < c.NCX:
                        kb.cp(sx.t[:, :, cc * 128:(cc + 1) * 128], src, [kb.pr[tb]], [sx.r], eng=("act" if cc % 2 else "dve"))
                    else:
                        g = cc - c.NCX
                        kb.cp(sb_.t[:, :, g * 128:(g + 1) * 128], src, [kb.pr[tb]], [sb_.r], eng=("act" if cc % 2 else "dve"))
                if cc >= c.NCX and ismain:
                    g = (cc - c.NCX) % 4
                    dstT = S["CT"] if isC else S["BT"]
                    m0 = t0 - (W - T)
                    kb.cp(dstT.t[:, g, m0:m0 + TT], ft.t[:], [ft.r], [dstT.rs[g]], eng="pool")
            kb.dma(io["xs_s"][t0:t0 + TT, :].rearrange("(b p) d -> p b d", p=128), sx.t[:], [sx.r], [S["xs_res"][tt]])
            kb.dma(io["B_s"][t0:t0 + TT, :].rearrange("(b p) d -> p b d", p=128), sb_.t[:], [sb_.r], [S["B_res"][tt]])
        kb.P.flush()
    with ExitStack() as st:
        sb = _sbf(nc, st)
        wdt = sb("wdt", [128, KC, c.H], F32)
        kb.dma(wdt.t[:], io["w_in"][:, c.o_dt:c.o_dt + c.H].rearrange("(kc p) n -> p kc n", p=128), [], [wdt.r])
        xf = [sb("xf%d" % i, [128, KC, 128], F32) for i in range(2)]
        dt = S["dt"]
        la = S["la"]
        for b in range(c.NW):
            x_ = xf[b % 2]
            kb.dma(x_.t[:], io["xTw"][:, 3 + b * 128:3 + (b + 1) * 128].rearrange("(kc p) n -> p kc n", p=128), [], [x_.r])
            pb = 6 + b % 2
            for kc in range(KC):
                kb.mm(kb.bank(pb, c.H), x_.t[:, kc, :], wdt.t[:, kc, :], kc == 0, kc == KC - 1, [x_.r, wdt.r], [kb.pr[pb]])
            kb.cp(dt.t[:, b, :], kb.bank(pb, c.H), [kb.pr[pb]], [dt.r], eng="act")
        vec = sb("vec", [128, 2, c.H], F32)
        kb.dma(vec.t[:, 0, :], io["dt_bias"].to_broadcast([128, c.H]), [], [vec.r])
        kb.dma(vec.t[:, 1, :], io["a_log"].to_broadcast([128, c.H]), [], [vec.r])
        vld = sb("vld", [128, c.NW], F32)
        kb.dma(vld.t[:], io["valid"], [], [vld.r])
        tmp = sb("sptmp", [128, c.NW, c.H], F32)
        NWH = [128, c.NW, c.H]
        kb.tt(dt.t[:], dt.t[:], vec.t[:, 0:1, :].to_broadcast(NWH), ALU.add, [dt.r, vec.r], [dt.r])
        kb.act(tmp.t[:], dt.t[:], AF.Abs, [dt.r], [tmp.r])
        kb.act(tmp.t[:], tmp.t[:], AF.Exp, [tmp.r], [tmp.r], scale=-1.0)
        kb.act(tmp.t[:], tmp.t[:], AF.Ln, [tmp.r], [tmp.r], bias=1.0)
        kb.ts(dt.t[:], dt.t[:], 0.0, None, ALU.max, None, [dt.r], [dt.r])
        kb.tt(dt.t[:], dt.t[:], tmp.t[:], ALU.add, [dt.r, tmp.r], [dt.r])
        kb.tt(dt.t[:], dt.t[:], vld.t[:].unsqueeze(2).to_broadcast(NWH), ALU.mult, [dt.r, vld.r], [dt.r])
        kb.act(vec.t[:, 1, :], vec.t[:, 1, :], AF.Exp, [vec.r], [vec.r])
        kb.ts(vec.t[:, 1, :], vec.t[:, 1, :], -1.0, None, ALU.mult, None, [vec.r], [vec.r])
        kb.tt(la.t[:], dt.t[:], vec.t[:, 1:2, :].to_broadcast(NWH), ALU.mult, [dt.r, vec.r], [la.r])
        kb.P.flush()


def phase2(kb, io, S):
    c = kb.c
    nc = kb.nc
    H, HG, GW, D = c.H, c.HG, c.GW, c.D
    with ExitStack() as st:
        sb = _sbf(nc, st)
        esel = sb("esel_sb", [H, H, 128], F32)
        kb.dma(esel.t[:], io["esel"].rearrange("k (h j) -> k h j", j=128), [], [esel.r])
        dsk = sb("dsk", [128, H], F32)
        kb.dma(dsk.t[:], io["d_skip"].to_broadcast([128, H]), [], [dsk.r])
        Hs = sb("Hs", [128, D], F32, 4)
        Hb = sb("Hb", [128, D], BF16, 4)
        for g in range(4):
            kb.memset(Hs.t[:, g * GW:(g + 1) * GW], 0.0, [], [Hs.rs[g]])
            kb.memset(Hb.t[:, g * GW:(g + 1) * GW], 0.0, [], [Hb.rs[g]], eng="pool")
        xs_t = [sb("xs_t%d" % i, [128, D], BF16) for i in range(2)]
        B_t = [sb("B_t%d" % i, [128, 4, 128], BF16) for i in range(2)]
        lcs = [sb("lcs%d" % i, [128, H], F32) for i in range(2)]
        sm = [sb("sm%d" % i, [128, 4, H], F32) for i in range(2)]
        xdd = [sb("xdd%d" % i, [128, D], BF16) for i in range(2)]
        xd = [sb("xd%d" % i, [128, D], BF16) for i in range(2)]
        lcT = [sb("lcT%d" % i, [H, 2, 128], F32) for i in range(2)]
        cbT = [sb("cbT%d" % i, [128, 128], F32) for i in range(2)]
        decT = [sb("decT%d" % i, [128, 4, 128], F32) for i in range(2)]
        LT = [sb("LT%d" % i, [128, 4, 128], BF16) for i in range(2)]
        t1 = [sb("t1_%d" % i, [128, GW], F32) for i in range(2)]
        t2 = [sb("t2_%d" % i, [128, GW], F32) for i in range(2)]
        tri, ones, identf, mneg = S["tri"], S["ones"], S["identf"], S["mneg"]
        dt, la, BT, CT, ytok = S["dt"], S["la"], S["BT"], S["CT"], S["ytok"]
        qi = 0
        gi = 0
        for cb in range(c.NW):
            main = cb >= c.NP
            m = cb - c.NP
            last = cb == c.NW - 1
            x_ = xs_t[cb % 2]
            kb.dma(x_.t[:], io["xs_s"][cb * 128:(cb + 1) * 128, :], S["xs_res"], [x_.r])
            x3 = x_.t[:].rearrange("p (h d) -> p h d", d=64)
            b_ = B_t[cb % 2]
            if not last:
                kb.dma(b_.t[:].rearrange("p g n -> p (g n)"), io["B_s"][cb * 128:(cb + 1) * 128, :], S["B_res"], [b_.r])
            la_c = la.t[:, cb, :]
            lc = lcs[cb % 2]
            s_ = sm[cb % 2]
            kb.mm(kb.bank(0, H), tri.t[:], la_c, True, True, [tri.r, la.r], [kb.pr[0]])
            kb.mm(kb.bank(0, H, off=H), ones.t[:], la_c, True, True, [ones.r, la.r], [kb.pr[0]])
            if main:
                kb.mm(kb.ps[0:H, 128:256], la_c, tri.t[:], True, True, [tri.r, la.r], [kb.pr[0]])
            kb.cp(lc.t[:], kb.bank(0, H), [kb.pr[0]], [lc.r])
            kb.tt(s_.t[:, 0, :], kb.bank(0, H, off=H), lc.t[:], ALU.subtract, [kb.pr[0], lc.r], [s_.r])
            kb.act(s_.t[:, 0, :], s_.t[:, 0, :], AF.Exp, [s_.r], [s_.r])
            kb.act(s_.t[:, 1, :], kb.bank(0, H, off=H), AF.Exp, [kb.pr[0]], [s_.r])
            kb.tt(s_.t[:, 2, :], dt.t[:, cb, :], s_.t[:, 0, :], ALU.mult, [dt.r, s_.r], [s_.r])
            xq = xdd[cb % 2]
            if not last:
                kb.tt(xq.t[:].rearrange("p (h d) -> p h d", d=64), x3,
                      s_.t[:, 2, :].unsqueeze(2).to_broadcast([128, H, 64]), ALU.mult, [x_.r, s_.r], [xq.r])
            if main:
                lt_ = lcT[cb % 2]
                kb.cp(lt_.t[:, 0, :], kb.ps[0:H, 128:256], [kb.pr[0]], [lt_.r])
                kb.ts(lt_.t[:, 1, :], kb.ps[0:H, 128:256], -1.0, None, ALU.mult, None, [kb.pr[0]], [lt_.r])
                kb.act(s_.t[:, 3, :], lc.t[:], AF.Exp, [lc.r], [s_.r])
                xd_ = xd[cb % 2]
                kb.tt(xd_.t[:].rearrange("p (h d) -> p h d", d=64), x3,
                      dt.t[:, cb, :].unsqueeze(2).to_broadcast([128, H, 64]), ALU.mult, [x_.r, dt.r], [xd_.r])
                mb = slice(m * 128, (m + 1) * 128)
                for g in range(4):
                    cb_ = cbT[gi % 2]
                    yb = 4 + gi % 2
                    ob = 6 + gi % 2
                    ta = t1[gi % 2]
                    tb_ = t2[gi % 2]
                    gi += 1
                    kb.mm(kb.bank(1, 128), BT.t[:, g, mb], CT.t[:, g, mb], True, True, [BT.rs[g], CT.rs[g]], [kb.pr[1]])
                    kb.cp(cb_.t[:], kb.bank(1, 128), [kb.pr[1]], [cb_.r], eng="act")
                    for q0 in range(0, HG, 4):
                        nq = min(4, HG - q0)
                        pb = 2 + qi % 2
                        dc_ = decT[qi % 2]
                        l_ = LT[qi % 2]
                        qi += 1
                        for j in range(nq):
                            h = g * HG + q0 + j
                            o_ = kb.bank(pb, 128, off=j * 128)
                            kb.mm(o_, esel.t[:, h, :], lt_.t[:, 0, :], True, False, [esel.r, lt_.r], [kb.pr[pb]])
                            kb.mm(o_, lt_.t[:, 1, :], esel.t[:, h, :], False, False, [esel.r, lt_.r], [kb.pr[pb]])
                            kb.mm(o_, identf.t[:], mneg.t[:], False, True, [identf.r, mneg.r], [kb.pr[pb]])
                        kb.act(dc_.t[:, 0:nq, :], kb.bank(pb, nq * 128).rearrange("p (q i) -> p q i", i=128), AF.Exp,
                               [kb.pr[pb]], [dc_.r])
                        kb.tt(l_.t[:, 0:nq, :], dc_.t[:, 0:nq, :], cb_.t[:].unsqueeze(1).to_broadcast([128, nq, 128]),
                              ALU.mult, [dc_.r, cb_.r], [l_.r])
                        for j in range(nq):
                            h = g * HG + q0 + j
                            kb.mm(kb.bank(yb, 64, off=(q0 + j) * 64), l_.t[:, j, :], xd_.t[:, h * 64:(h + 1) * 64], True, True,
                                  [l_.r, xd_.r], [kb.pr[yb]])
                    gs = slice(g * GW, (g + 1) * GW)
                    hs = slice(g * HG, (g + 1) * HG)
                    kb.mm(kb.bank(ob, GW), CT.t[:, g, mb], Hb.t[:, gs], True, True, [CT.rs[g], Hb.rs[g]], [kb.pr[ob]])
                    kb.tt(ta.t[:].rearrange("p (h d) -> p h d", d=64), kb.bank(ob, GW).rearrange("p (h d) -> p h d", d=64),
                          s_.t[:, 3, hs].unsqueeze(2).to_broadcast([128, HG, 64]), ALU.mult, [kb.pr[ob], s_.r], [ta.r])
                    kb.tt(ta.t[:], ta.t[:], kb.bank(yb, GW), ALU.add, [ta.r, kb.pr[yb]], [ta.r])
                    kb.tt(tb_.t[:].rearrange("p (h d) -> p h d", d=64), x3[:, hs, :],
                          dsk.t[:, hs].unsqueeze(2).to_broadcast([128, HG, 64]), ALU.mult, [x_.r, dsk.r], [tb_.r], eng="pool")
                    kb.tt(ytok.t[:, m, gs], ta.t[:], tb_.t[:], ALU.add, [ta.r, tb_.r], [ytok.rs[m]])
            if not last:
                for g in range(4):
                    gs = slice(g * GW, (g + 1) * GW)
                    hs = slice(g * HG, (g + 1) * HG)
                    sbk = 6 + gi % 2
                    gi += 1
                    kb.mm(kb.bank(sbk, GW), b_.t[:, g, :], xq.t[:, gs], True, True, [b_.r, xq.r], [kb.pr[sbk]])
                    hv = Hs.t[:, gs].rearrange("p (h d) -> p h d", d=64)
                    kb.tt(hv, hv, s_.t[:, 1, hs].unsqueeze(2).to_broadcast([128, HG, 64]), ALU.mult, [Hs.rs[g], s_.r], [Hs.rs[g]])
                    kb.tt(Hs.t[:, gs], Hs.t[:, gs], kb.bank(sbk, GW), ALU.add, [Hs.rs[g], kb.pr[sbk]], [Hs.rs[g]])
                    if cb >= c.NP - 1:
                        kb.cp(Hb.t[:, gs], Hs.t[:, gs], [Hs.rs[g]], [Hb.rs[g]], eng="act")
        kb.P.flush()


def phase3(kb, io, S):
    c = kb.c
    nc = kb.nc
    GW, D = c.GW, c.D
    with ExitStack() as st:
        sb = _sbf(nc, st)
        wz = [sb("wz%d" % i, [128, c.KC, GW], BF16) for i in range(2)]
        ng = sb("ng", [128, D], F32)
        kb.dma(ng.t[:], io["ssd_norm_g"].to_broadcast([128, D]), [], [ng.r])
        sz = [sb("sz%d" % i, [128, GW], F32) for i in range(2)]
        jk = sb("jk", [128, GW], F32)
        ss = [sb("ss%d" % i, [128, 2], F32) for i in range(2)]
        yn = [sb("yn%d" % i, [128, GW], BF16) for i in range(2)]
        ytok, ident = S["ytok"], S["ident"]
        xm = load_xm(kb, io, sb)
        ynT = sb("ynT", [128, c.KC, c.T], BF16)
        NT = GW // 128
        it = 0
        for g in range(4):
            w = wz[g % 2]
            kb.dma(w.t[:], io["w_in"][:, c.o_z + g * GW:c.o_z + (g + 1) * GW].rearrange("(kc p) n -> p kc n", p=128),
                   [], [w.r], eng="pool")
            for b in range(c.NB):
                pb = it % 4
                z_ = sz[it % 2]
                s_ = ss[it % 2]
                y_ = yn[it % 2]
                tb = 4 + it % 2
                it += 1
                tok = slice(128 + b * 128, 128 + (b + 1) * 128)
                for kc in range(c.KC):
                    kb.mm(kb.bank(pb, GW), xm.t[:, kc, tok], w.t[:, kc, :], kc == 0, kc == c.KC - 1, [xm.rs[kc], w.r], [kb.pr[pb]])
                kb.act(z_.t[:], kb.bank(pb, GW), AF.Silu, [kb.pr[pb]], [z_.r])
                kb.tt(z_.t[:], z_.t[:], ytok.t[:, b, g * GW:(g + 1) * GW], ALU.mult, [z_.r, ytok.rs[b]], [z_.r])
                kb.memset(s_.t[:], 0.0, [], [s_.r])
                kb.act(jk.t[:], z_.t[:], AF.Square, [z_.r, s_.r], [jk.r, s_.r], accum=s_.t[:, 0:1])
                kb.ts(s_.t[:, 1:2], s_.t[:, 0:1], 1.0 / GW, c.RMS_EPS, ALU.mult, ALU.add, [s_.r], [s_.r])
                kb.act(s_.t[:, 1:2], s_.t[:, 1:2], AF.Sqrt, [s_.r], [s_.r])
                kb.P.op("dve", (lambda o, i: (lambda e: e.reciprocal(out=o, in_=i)))(s_.t[:, 1:2], s_.t[:, 1:2]), [s_.r], [s_.r])
                kb.stt(y_.t[:], z_.t[:], s_.t[:, 1:2], ng.t[:, g * GW:(g + 1) * GW], ALU.mult, ALU.mult, [z_.r, s_.r, ng.r], [y_.r])
                for j in range(NT):
                    kb.tr(kb.bankbf(tb)[:, j * 128:(j + 1) * 128], y_.t[:, j * 128:(j + 1) * 128], ident.t[:],
                          [y_.r, ident.r], [kb.pr[tb]])
                kb.cp(ynT.t[:, g * NT:(g + 1) * NT, b * 128:(b + 1) * 128],
                      kb.bankbf(tb)[:, 0:NT * 128].rearrange("p (j t) -> p j t", t=128), [kb.pr[tb]], [ynT.r])
        kb.dma(io["ynT_s"], ynT.t[:], [ynT.r], [Res("ynTs")])
        kb.P.flush()


def phase4(kb, io, S):
    c = kb.c
    nc = kb.nc
    T, NB, HQ, HKV, GRP = c.T, c.NB, c.HQ, c.HKV, c.GRP
    with ExitStack() as st:
        sb = _sbf(nc, st)
        ident = S["ident"]
        xm = load_xm(kb, io, sb)
        oswaT = sb("oswaT", [128, c.QD // 128, T], BF16)
        qT = sb("qT", [64, HQ, T], BF16, HQ)
        kT = sb("kT", [64, HKV, T + 128], BF16, HKV)
        vt = sb("vt", [128, NB + 1, c.KVD], BF16)
        wq = [sb("wq%d" % i, [128, c.KC, 128], BF16) for i in range(2)]
        wv = sb("wv", [128, c.KC, c.KVD], BF16)
        bi = 0
        for pr_ in range((HQ + HKV) // 2):
            isq = pr_ < HQ // 2
            col0 = (c.o_q + pr_ * 128) if isq else (c.o_k + (pr_ - HQ // 2) * 128)
            w = wq[pr_ % 2]
            kb.dma(w.t[:], io["w_in"][:, col0:col0 + 128].rearrange("(kc p) n -> p kc n", p=128), [], [w.r], eng="pool")
            for half in range(2):
                hh = (pr_ * 2 + half) if isq else ((pr_ - HQ // 2) * 2 + half)
                t0 = 128 if isq else 0
                while t0 < T + 128:
                    n = min(512, T + 128 - t0)
                    b = bi % 4
                    bi += 1
                    for kc in range(c.KC):
                        kb.mm(kb.ps[0:64, b * 512:b * 512 + n], w.t[:, kc, half * 64:(half + 1) * 64], xm.t[:, kc, t0:t0 + n],
                              kc == 0, kc == c.KC - 1, [w.r, xm.rs[kc]], [kb.pr[b]])
                    if isq:
                        kb.act(qT.t[:, hh, t0 - 128:t0 - 128 + n], kb.ps[0:64, b * 512:b * 512 + n], AF.Copy,
                               [kb.pr[b]], [qT.rs[hh]], scale=0.125)
                    else:
                        kb.cp(kT.t[:, hh, t0:t0 + n], kb.ps[0:64, b * 512:b * 512 + n], [kb.pr[b]], [kT.rs[hh]])
                    t0 += n
        kb.dma(wv.t[:], io["w_in"][:, c.o_v:c.o_v + c.KVD].rearrange("(kc p) n -> p kc n", p=128), [], [wv.r], eng="pool")
        for b in range(NB + 1):
            pb = bi % 4
            bi += 1
            for kc in range(c.KC):
                kb.mm(kb.bank(pb, c.KVD), xm.t[:, kc, b * 128:(b + 1) * 128], wv.t[:, kc, :], kc == 0, kc == c.KC - 1,
                      [xm.rs[kc], wv.r], [kb.pr[pb]])
            kb.cp(vt.t[:, b, :], kb.bank(pb, c.KVD), [kb.pr[pb]], [vt.r], eng="act")
        bm = sb("bm", [128, 2, HQ, 256], F32)
        msk = sb("msk", [128, 2, 256], F32)
        kb.dma(bm.t[:, 0, :, :], io["swa_bias"].rearrange("h p j -> p h j"), [], [bm.r])
        kb.dma(bm.t[:, 1, :, :], io["swa_bias"].rearrange("h p j -> p h j"), [], [bm.r])
        kb.dma(msk.t[:], io["swa_mask"].rearrange("m p j -> p m j"), [], [msk.r])
        for v in range(2):
            kb.tt(bm.t[:, v, :, :], bm.t[:, v, :, :], msk.t[:, v:v + 1, :].to_broadcast([128, HQ, 256]), ALU.add,
                  [bm.r, msk.r], [bm.r])
        snk = sb("snk", [128, HQ], F32)
        kb.dma(snk.t[:], io["swa_sinks"].to_broadcast([128, HQ]), [], [snk.r])
        Sm = [sb("Sm%d" % i, [128, GRP, 256], F32) for i in range(2)]
        Pm = [sb("Pm%d" % i, [128, GRP, 256], BF16) for i in range(2)]
        PT = [sb("PT%d" % i, [128, GRP * 2, 128], BF16) for i in range(2)]
        st_ = [sb("st%d" % i, [128, 6, GRP], F32) for i in range(2)]
        rden = sb("rden", [128, HQ], F32)
        osb = [sb("osb%d" % i, [128, c.QD], BF16) for i in range(2)]
        it = 0
        NQ = c.QD // 128
        for b in range(NB):
            var = 0 if b == 0 else 1
            for hk in range(HKV):
                s2 = (it % 2) * 2
                sm_ = Sm[it % 2]
                pm_ = Pm[it % 2]
                pt_ = PT[it % 2]
                s_ = st_[it % 2]
                tbk = 4 + it % 2
                it += 1
                hsl = slice(hk * GRP, (hk + 1) * GRP)
                for g in range(GRP):
                    h = hk * GRP + g
                    col = s2 * 512 + g * 256
                    kb.mm(kb.ps[:, col:col + 256], qT.t[:, h, b * 128:(b + 1) * 128], kT.t[:, hk, b * 128:b * 128 + 256],
                          True, True, [qT.rs[h], kT.rs[hk]], [kb.pr[s2 + (g * 256) // 512]])
                nbk = (GRP * 256 + 511) // 512
                prs = [kb.pr[s2 + i] for i in range(nbk)]
                kb.tt(sm_.t[:], kb.ps[:, s2 * 512:s2 * 512 + GRP * 256].rearrange("p (g j) -> p g j", j=256),
                      bm.t[:, var, hsl, :], ALU.add, prs + [bm.r], [sm_.r])
                kb.red(s_.t[:, 0, :], sm_.t[:], ALU.max, [sm_.r], [s_.r])
                kb.tt(s_.t[:, 0, :], s_.t[:, 0, :], snk.t[:, hsl], ALU.max, [s_.r, snk.r], [s_.r])
                kb.ts(s_.t[:, 1, :], s_.t[:, 0, :], -1.0, None, ALU.mult, None, [s_.r], [s_.r])
                kb.memset(s_.t[:, 2, :], 0.0, [], [s_.r])
                for g in range(GRP):
                    kb.act(pm_.t[:, g, :], sm_.t[:, g, :], AF.Exp, [sm_.r, s_.r], [pm_.r, s_.r],
                           bias=s_.t[:, 1, g:g + 1], accum=s_.t[:, 2, g:g + 1])
                kb.tt(s_.t[:, 3, :], snk.t[:, hsl], s_.t[:, 0, :], ALU.subtract, [s_.r, snk.r], [s_.r])
                kb.act(s_.t[:, 3, :], s_.t[:, 3, :], AF.Exp, [s_.r], [s_.r])
                kb.tt(s_.t[:, 4, :], s_.t[:, 2, :], s_.t[:, 3, :], ALU.add, [s_.r], [s_.r])
                kb.P.op("dve", (lambda o, i: (lambda e: e.reciprocal(out=o, in_=i)))(rden.t[:, hsl], s_.t[:, 4, :]),
                        [s_.r], [rden.r])
                for g in range(GRP):
                    for jb in range(2):
                        kb.tr(kb.bankbf(tbk)[:, (g * 2 + jb) * 128:(g * 2 + jb + 1) * 128], pm_.t[:, g, jb * 128:(jb + 1) * 128],
                              ident.t[:], [pm_.r, ident.r], [kb.pr[tbk]])
                kb.cp(pt_.t[:], kb.bankbf(tbk)[:, 0:GRP * 256].rearrange("p (q i) -> p q i", i=128), [kb.pr[tbk]], [pt_.r],
                      eng="act")
                for g in range(GRP):
                    h = hk * GRP + g
                    ob = 6 + (h * 64) // 512
                    for jb in range(2):
                        kb.mm(kb.ps[:, 6 * 512 + h * 64:6 * 512 + (h + 1) * 64], pt_.t[:, g * 2 + jb, :],
                              vt.t[:, b + jb, hk * 64:(hk + 1) * 64], jb == 0, jb == 1, [pt_.r, vt.r], [kb.pr[ob]])
            o_ = osb[b % 2]
            nob = (c.QD + 511) // 512
            kb.tt(o_.t[:].rearrange("p (h d) -> p h d", d=64), kb.ps[:, 6 * 512:6 * 512 + c.QD].rearrange("p (h d) -> p h d", d=64),
                  rden.t[:].unsqueeze(2).to_broadcast([128, HQ, 64]), ALU.mult, [kb.pr[6 + i] for i in range(nob)] + [rden.r], [o_.r])
            tbk = 4 + it % 2
            for j in range(NQ):
                kb.tr(kb.bankbf(tbk)[:, j * 128:(j + 1) * 128], o_.t[:, j * 128:(j + 1) * 128], ident.t[:], [o_.r, ident.r], [kb.pr[tbk]])
            kb.cp(oswaT.t[:, :, b * 128:(b + 1) * 128], kb.bankbf(tbk)[:, 0:NQ * 128].rearrange("p (j t) -> p j t", t=128),
                  [kb.pr[tbk]], [oswaT.r], eng="act")
        kb.dma(io["oswaT_s"], oswaT.t[:], [oswaT.r], [Res("oswaTs")])
        kb.P.flush()


def phase5(kb, io, S):
    c = kb.c
    nc = kb.nc
    T, NB, XAH, XAD, XDC, XD, MEM = c.T, c.NB, c.XAH, c.XAD, c.XDC, c.XD, c.MEM
    NXC = XD // 128
    NMB = MEM // 128
    with ExitStack() as st:
        sb = _sbf(nc, st)
        ident = S["ident"]
        xm = load_xm(kb, io, sb)
        oxaT = sb("oxaT", [128, XD // 128, T], BF16)
        mT = sb("mT", [128, c.KC, MEM], BF16)
        kb.dma(mT.t[:], io["memT"].rearrange("(kc p) m -> p kc m", p=128), [], [mT.r], eng="pool")
        mkT = sb("mkT", [128, NXC, MEM], BF16)
        mv = sb("mv", [128, NMB, XD], BF16)
        qx = sb("qx", [128, NXC, T], BF16, NXC)
        wc = [sb("wc%d" % i, [128, c.KC, 128], BF16) for i in range(2)]
        bi = 0
        for j in range(NXC):
            w = wc[bi % 2]
            kb.dma(w.t[:], io["w_mem_kv"][:, j * 128:(j + 1) * 128].rearrange("(kc p) n -> p kc n", p=128), [], [w.r], eng="pool")
            b = bi % 4
            bi += 1
            for kc in range(c.KC):
                kb.mm(kb.bank(b, MEM), w.t[:, kc, :], mT.t[:, kc, :], kc == 0, kc == c.KC - 1, [w.r, mT.r], [kb.pr[b]])
            kb.cp(mkT.t[:, j, :], kb.bank(b, MEM), [kb.pr[b]], [mkT.r], eng="act")
        wm = [sb("wm%d" % i, [128, c.KC, 512], BF16) for i in range(2)]
        ci = 0
        for c0 in range(0, XD, 512):
            n = min(512, XD - c0)
            w = wm[ci % 2]
            ci += 1
            kb.dma(w.t[:, :, 0:n], io["w_mem_kv"][:, XD + c0:XD + c0 + n].rearrange("(kc p) n -> p kc n", p=128), [], [w.r], eng="pool")
            for mb in range(NMB):
                b = bi % 4
                bi += 1
                for kc in range(c.KC):
                    kb.mm(kb.bank(b, n), mT.t[:, kc, mb * 128:(mb + 1) * 128], w.t[:, kc, 0:n], kc == 0, kc == c.KC - 1,
                          [w.r, mT.r], [kb.pr[b]])
                kb.cp(mv.t[:, mb, c0:c0 + n], kb.bank(b, n), [kb.pr[b]], [mv.r])
        scale = float(XAD) ** -0.5
        for j in range(NXC):
            w = wc[bi % 2]
            kb.dma(w.t[:], io["w_in"][:, c.o_qx + j * 128:c.o_qx + (j + 1) * 128].rearrange("(kc p) n -> p kc n", p=128),
                   [], [w.r], eng="pool")
            t0 = 0
            while t0 < T:
                n = min(512, T - t0)
                b = bi % 4
                bi += 1
                for kc in range(c.KC):
                    kb.mm(kb.bank(b, n), w.t[:, kc, :], xm.t[:, kc, 128 + t0:128 + t0 + n], kc == 0, kc == c.KC - 1,
                          [w.r, xm.rs[kc]], [kb.pr[b]])
                kb.act(qx.t[:, j, t0:t0 + n], kb.bank(b, n), AF.Copy, [kb.pr[b]], [qx.rs[j]], scale=scale)
                t0 += n
        Sx = [sb("Sx%d" % i, [128, MEM], F32) for i in range(2)]
        Px = [sb("Px%d" % i, [128, MEM], BF16) for i in range(2)]
        PTx = [sb("PTx%d" % i, [128, NMB, 128], BF16) for i in range(2)]
        sx = [sb("sx%d" % i, [128, 4], F32) for i in range(2)]
        rdx = sb("rdx", [128, XAH], F32)
        ox = [sb("ox%d" % i, [128, XD], BF16) for i in range(2)]
        it = 0
        nob = (XD + 511) // 512
        for b in range(NB):
            for h in range(XAH):
                pb = it % 2
                s_ = sx[it % 2]
                p_ = Px[it % 2]
                pt_ = PTx[it % 2]
                tbk = 4 + it % 2
                it += 1
                for dc in range(XDC):
                    kb.mm(kb.bank(pb, MEM), qx.t[:, h * XDC + dc, b * 128:(b + 1) * 128], mkT.t[:, h * XDC + dc, :],
                          dc == 0, dc == XDC - 1, [qx.rs[h * XDC + dc], mkT.r], [kb.pr[pb]])
                kb.red(s_.t[:, 0:1], kb.bank(pb, MEM), ALU.max, [kb.pr[pb]], [s_.r])
                kb.ts(s_.t[:, 1:2], s_.t[:, 0:1], -1.0, None, ALU.mult, None, [s_.r], [s_.r])
                kb.memset(s_.t[:, 2:3], 0.0, [], [s_.r])
                kb.act(p_.t[:], kb.bank(pb, MEM), AF.Exp, [kb.pr[pb], s_.r], [p_.r, s_.r], bias=s_.t[:, 1:2], accum=s_.t[:, 2:3])
                kb.P.op("dve", (lambda o, i: (lambda e: e.reciprocal(out=o, in_=i)))(rdx.t[:, h:h + 1], s_.t[:, 2:3]),
                        [s_.r], [rdx.r])
                for mb in range(NMB):
                    kb.tr(kb.bankbf(tbk)[:, mb * 128:(mb + 1) * 128], p_.t[:, mb * 128:(mb + 1) * 128], ident.t[:],
                          [p_.r, ident.r], [kb.pr[tbk]])
                kb.cp(pt_.t[:], kb.bankbf(tbk)[:, 0:NMB * 128].rearrange("p (q i) -> p q i", i=128), [kb.pr[tbk]], [pt_.r])
                ob = 6 + (h * XAD) // 512
                for mb in range(NMB):
                    kb.mm(kb.ps[:, 6 * 512 + h * XAD:6 * 512 + (h + 1) * XAD], pt_.t[:, mb, :], mv.t[:, mb, h * XAD:(h + 1) * XAD],
                          mb == 0, mb == NMB - 1, [pt_.r, mv.r], [kb.pr[ob]])
            o_ = ox[b % 2]
            kb.tt(o_.t[:].rearrange("p (h d) -> p h d", d=XAD), kb.ps[:, 6 * 512:6 * 512 + XD].rearrange("p (h d) -> p h d", d=XAD),
                  rdx.t[:].unsqueeze(2).to_broadcast([128, XAH, XAD]), ALU.mult, [kb.pr[6 + i] for i in range(nob)] + [rdx.r], [o_.r])
            tbk = 4 + it % 2
            for j in range(NXC):
                kb.tr(kb.bankbf(tbk)[:, j * 128:(j + 1) * 128], o_.t[:, j * 128:(j + 1) * 128], ident.t[:], [o_.r, ident.r], [kb.pr[tbk]])
            kb.cp(oxaT.t[:, :, b * 128:(b + 1) * 128], kb.bankbf(tbk)[:, 0:NXC * 128].rearrange("p (j t) -> p j t", t=128),
                  [kb.pr[tbk]], [oxaT.r], eng="act")
        kb.dma(io["oxaT_s"], oxaT.t[:], [oxaT.r], [Res("oxaTs")])
        kb.P.flush()


def phase6(kb, io, S):
    c = kb.c
    nc = kb.nc
    T, D, KC = c.T, c.D, c.KC
    NQ = c.QD // 128
    NX = c.XD // 128
    with ExitStack() as st:
        sb = _sbf(nc, st)
        xm = load_xm(kb, io, sb)
        ynT = sb("ynT6", [128, KC, T], BF16)
        oswaT = sb("oswaT6", [128, NQ, T], BF16)
        oxaT = sb("oxaT6", [128, NX, T], BF16)
        mg = sb("mgT6", [128, KC, T], BF16)
        kb.dma(ynT.t[:], io["ynT_s"], [], [ynT.r])
        kb.dma(oswaT.t[:], io["oswaT_s"], [], [oswaT.r])
        kb.dma(oxaT.t[:], io["oxaT_s"], [], [oxaT.r])
        wso = [sb("wso%d" % i, [128, KC, 128], BF16) for i in range(2)]
        wwo = [sb("wwo%d" % i, [128, NQ, 128], BF16) for i in range(2)]
        wxo = [sb("wxo%d" % i, [128, NX, 128], BF16) for i in range(2)]
        wg = [sb("wg%d" % i, [128, 3, KC, 128], BF16, 3) for i in range(2)]
        sg = [sb("sg%d" % i, [128, 3, 512], F32) for i in range(2)]
        ma = [sb("ma%d" % i, [128, 512], F32) for i in range(2)]
        mb_ = [sb("mb%d" % i, [128, 512], F32) for i in range(2)]
        it = 0
        for dc in range(D // 128):
            cs = slice(dc * 128, (dc + 1) * 128)
            a, b_, x_, g_ = wso[dc % 2], wwo[dc % 2], wxo[dc % 2], wg[dc % 2]
            kb.dma(a.t[:], io["w_ssd_o"][:, cs].rearrange("(kc p) n -> p kc n", p=128), [], [a.r], eng="pool")
            kb.dma(b_.t[:], io["w_swa_o"][:, cs].rearrange("(kc p) n -> p kc n", p=128), [], [b_.r], eng="pool")
            kb.dma(x_.t[:], io["w_xa_o"][:, cs].rearrange("(kc p) n -> p kc n", p=128), [], [x_.r], eng="pool")
            for i in range(3):
                col = c.o_g + i * D + dc * 128
                kb.dma(g_.t[:, i, :, :], io["w_in"][:, col:col + 128].rearrange("(kc p) n -> p kc n", p=128), [], [g_.rs[i]], eng="pool")
            t0 = 0
            while t0 < T:
                n = min(512, T - t0)
                ts_ = slice(t0, t0 + n)
                s_ = sg[it % 2]
                m1 = ma[it % 2]
                m2 = mb_[it % 2]
                it += 1
                for kc in range(KC):
                    kb.mm(kb.bank(0, n), a.t[:, kc, :], ynT.t[:, kc, ts_], kc == 0, kc == KC - 1, [a.r, ynT.r], [kb.pr[0]])
                for kc in range(NQ):
                    kb.mm(kb.bank(1, n), b_.t[:, kc, :], oswaT.t[:, kc, ts_], kc == 0, kc == NQ - 1, [b_.r, oswaT.r], [kb.pr[1]])
                for kc in range(NX):
                    kb.mm(kb.bank(2, n), x_.t[:, kc, :], oxaT.t[:, kc, ts_], kc == 0, kc == NX - 1, [x_.r, oxaT.r], [kb.pr[2]])
                for i in range(3):
                    for kc in range(KC):
                        kb.mm(kb.bank(3 + i, n), g_.t[:, i, kc, :], xm.t[:, kc, 128 + t0:128 + t0 + n], kc == 0, kc == KC - 1,
                              [g_.rs[i], xm.rs[kc]], [kb.pr[3 + i]])
                    kb.act(s_.t[:, i, 0:n], kb.bank(3 + i, n), AF.Sigmoid, [kb.pr[3 + i]], [s_.r])
                kb.tt(m1.t[:, 0:n], s_.t[:, 0, 0:n], kb.bank(0, n), ALU.mult, [s_.r, kb.pr[0]], [m1.r])
                kb.tt(m2.t[:, 0:n], s_.t[:, 1, 0:n], kb.bank(1, n), ALU.mult, [s_.r, kb.pr[1]], [m2.r])
                kb.tt(m1.t[:, 0:n], m1.t[:, 0:n], m2.t[:, 0:n], ALU.add, [m1.r, m2.r], [m1.r])
                kb.tt(m2.t[:, 0:n], s_.t[:, 2, 0:n], kb.bank(2, n), ALU.mult, [s_.r, kb.pr[2]], [m2.r])
                kb.tt(mg.t[:, dc, ts_], m1.t[:, 0:n], m2.t[:, 0:n], ALU.add, [m1.r, m2.r], [mg.r])
                t0 += n
        kb.dma(io["mgT_s"], mg.t[:], [mg.r], [Res("mgTs")])
        kb.P.flush()


def _layernorm(kb, sb_small, x_ap, xr, out_ap, outr, gb, D, eps, jk):
    s_ = sb_small
    kb.red(s_.t[:, 0:1], x_ap, ALU.add, [xr], [s_.r])
    kb.ts(s_.t[:, 1:2], s_.t[:, 0:1], -1.0 / D, None, ALU.mult, None, [s_.r], [s_.r])
    kb.ts(x_ap, x_ap, s_.t[:, 1:2], None, ALU.add, None, [xr, s_.r], [xr])
    kb.memset(s_.t[:, 2:3], 0.0, [], [s_.r])
    kb.act(jk.t[:], x_ap, AF.Square, [xr, s_.r], [jk.r, s_.r], accum=s_.t[:, 2:3])
    kb.ts(s_.t[:, 3:4], s_.t[:, 2:3], 1.0 / D, eps, ALU.mult, ALU.add, [s_.r], [s_.r])
    kb.act(s_.t[:, 3:4], s_.t[:, 3:4], AF.Sqrt, [s_.r], [s_.r])
    kb.P.op("dve", (lambda o, i: (lambda e: e.reciprocal(out=o, in_=i)))(s_.t[:, 3:4], s_.t[:, 3:4]), [s_.r], [s_.r])
    kb.stt(x_ap, x_ap, s_.t[:, 3:4], gb.t[:, 0, :], ALU.mult, ALU.mult, [xr, s_.r, gb.r], [xr])
    kb.tt(out_ap, x_ap, gb.t[:, 1, :], ALU.add, [xr, gb.r], [outr] if outr is not xr else [xr])


def phase7(kb, io, S):
    c = kb.c
    nc = kb.nc
    T, D, KC, NB, E, CAP = c.T, c.D, c.KC, c.NB, c.E, c.CAP
    with ExitStack() as st:
        sb = _sbf(nc, st)
        acc, ident, identf = S["acc"], S["ident"], S["identf"]
        tris, ones, iota = S["tris"], S["ones"], S["iota"]
        dint, wk = S["dint"], S["wk"]
        mg = sb("mgT7", [128, KC, T], BF16)
        kb.dma(mg.t[:], io["mgT_s"], [], [mg.r])
        wo = [sb("wo%d" % i, [128, KC, 512], BF16) for i in range(2)]
        xt = [sb("xt%d" % i, [128, 512], F32) for i in range(2)]
        it = 0
        ci = 0
        for c0 in range(0, D, 512):
            n = min(512, D - c0)
            w = wo[ci % 2]
            ci += 1
            kb.dma(w.t[:, :, 0:n], io["w_out"][:, c0:c0 + n].rearrange("(kc p) n -> p kc n", p=128), [], [w.r], eng="pool")
            for b in range(NB):
                pb = it % 4
                x_ = xt[it % 2]
                it += 1
                kb.dma(x_.t[:, 0:n], io["xtok"][b * 128:(b + 1) * 128, c0:c0 + n], [], [x_.r])
                for kc in range(KC):
                    kb.mm(kb.bank(pb, n), mg.t[:, kc, b * 128:(b + 1) * 128], w.t[:, kc, 0:n], kc == 0, kc == KC - 1,
                          [mg.r, w.r], [kb.pr[pb]])
                kb.stt(acc.t[:, b, c0:c0 + n], x_.t[:, 0:n], c.ALPHA, kb.bank(pb, n), ALU.mult, ALU.add,
                       [x_.r, kb.pr[pb]], [acc.rs[b]])
        kb.P.flush()
    import os
    K7 = int(os.environ.get("K7", "9"))
    if K7 < 2:
        return
    with ExitStack() as st:
        sb = _sbf(nc, st)
        gb = sb("gb1", [128, 2, D], F32)
        kb.dma(gb.t[:, 0, :], io["ln1_g"].to_broadcast([128, D]), [], [gb.r])
        kb.dma(gb.t[:, 1, :], io["ln1_b"].to_broadcast([128, D]), [], [gb.r])
        hTb = [sb("hTb%d" % i, [128, KC, 128], BF16) for i in range(2)]
        sml = [sb("sml%d" % i, [128, 4], F32) for i in range(2)]
        jk = sb("jk7", [128, D], F32)
        hbf = [sb("hbf%d" % i, [128, D], BF16) for i in range(2)]
        lbf = [sb("lbf%d" % i, [128, D], BF16) for i in range(2)]
        lTb = [sb("lTb%d" % i, [128, KC, 128], BF16) for i in range(2)]
        rw = sb("rw", [128, KC, E], F32)
        kb.dma(rw.t[:], io["router_w"].rearrange("(kc p) e -> p kc e", p=128), [], [rw.r])
        rwh = sb("rwh", [128, KC, E], BF16)
        rwl = sb("rwl", [128, KC, E], BF16)
        kb.cp(rwh.t[:], rw.t[:], [rw.r], [rwh.r], eng="act")
        kb.tt(rwl.t[:], rw.t[:], rwh.t[:], ALU.subtract, [rw.r, rwh.r], [rwl.r])
        rbb = sb("rbb", [128, E], F32)
        kb.dma(rbb.t[:], io["router_bias"].to_broadcast([128, E]), [], [rbb.r])
        eoff = sb("eoff", [128, E], F32)
        kb.ts(eoff.t[:], iota.t[:], float(CAP), None, ALU.mult, None, [iota.r], [eoff.r])
        base = sb("base", [128, E], F32)
        kb.memset(base.t[:], 0.0, [], [base.r])
        R = [sb("rt%d" % i, [128, 8, E], F32) for i in range(2)]
        r8 = [sb("r8_%d" % i, [128, 8, 8], F32) for i in range(2)]
        idx8 = [sb("idx8_%d" % i, [128, 8], U32) for i in range(2)]
        dself = [sb("dself%d" % i, [128, 8], F32) for i in range(2)]
        for b in range(NB):
            ab = acc.t[:, b, :]
            ar = acc.rs[b]
            _layernorm(kb, sml[b % 2], ab, ar, ab, ar, gb, D, c.LN_EPS, jk)
            hb = hbf[b % 2]
            lb = lbf[b % 2]
            kb.cp(hb.t[:], ab, [ar], [hb.r], eng="act")
            kb.tt(lb.t[:], ab, hb.t[:], ALU.subtract, [ar, hb.r], [lb.r])
            hq = hTb[b % 2]
            lq = lTb[b % 2]
            ti = 0
            for src_, dst_ in ((hb, hq), (lb, lq)):
                for k0 in range(0, KC, 8):
                    nk = min(8, KC - k0)
                    pb = ti % 2
                    ti += 1
                    for j in range(nk):
                        kb.tr(kb.bankbf(pb)[:, j * 128:(j + 1) * 128], src_.t[:, (k0 + j) * 128:(k0 + j + 1) * 128], ident.t[:],
                              [src_.r, ident.r], [kb.pr[pb]])
                    kb.cp(dst_.t[:, k0:k0 + nk, :], kb.bankbf(pb)[:, 0:nk * 128].rearrange("p (j t) -> p j t", t=128),
                          [kb.pr[pb]], [dst_.r], eng=("act" if ti % 2 else "dve"))
            kb.dma(io["h1T_s"][:, :, b * 128:(b + 1) * 128], hq.t[:], [hq.r], [Res("h1Ts")])
            n3 = 3 * KC
            i3 = 0
            for kc in range(KC):
                for (l_, r_) in ((hq, rwh), (hq, rwl), (lq, rwh)):
                    kb.mm(kb.bank(2, E), l_.t[:, kc, :], r_.t[:, kc, :], i3 == 0, i3 == n3 - 1, [l_.r, r_.r], [kb.pr[2]])
                    i3 += 1
            if K7 < 3:
                continue
            r = R[b % 2]
            rr = r.r
            q = r8[b % 2]
            sc, sel, tmp, selm, M, wn, dest, oh = (r.t[:, i, :] for i in range(8))
            v3 = lambda ap: ap.rearrange("p (g e) -> p g e", e=8)
            kb.act(sc, kb.bank(2, E), AF.Sigmoid, [kb.pr[2]], [rr])
            kb.tt(sel, sc, rbb.t[:], ALU.add, [rr, rbb.r], [rr])
            kb.red(q.t[:, 0, :], v3(sel), ALU.max, [rr], [q.r])
            kb.tt(v3(tmp), v3(sel), q.t[:, 0, :].unsqueeze(2).to_broadcast([128, 8, 8]), ALU.is_equal, [rr, q.r], [rr])
            kb.stt(tmp, tmp, -1e9, sel, ALU.mult, ALU.add, [rr], [rr])
            kb.red(q.t[:, 1, :], v3(tmp), ALU.max, [rr], [q.r])
            kb.tt(q.t[:, 2, :], q.t[:, 0, :], q.t[:, 1, :], ALU.add, [q.r], [q.r])
            kb.P.op("dve", (lambda o, i: (lambda e: e.max(out=o, in_=i)))(q.t[:, 3, :], q.t[:, 2, :]), [q.r], [q.r])
            kb.ts(q.t[:, 4, :], q.t[:, 2, :], q.t[:, 3, 3:4], None, ALU.is_ge, None, [q.r], [q.r])
            kb.ts(q.t[:, 5, :], q.t[:, 4, :], 1e9, -1e9, ALU.mult, ALU.add, [q.r], [q.r])
            kb.tt(v3(selm), v3(sel), q.t[:, 4, :].unsqueeze(2).to_broadcast([128, 8, 8]), ALU.mult, [rr, q.r], [rr])
            kb.tt(v3(selm), v3(selm), q.t[:, 5, :].unsqueeze(2).to_broadcast([128, 8, 8]), ALU.add, [rr, q.r], [rr])
            kb.P.op("dve", (lambda o, i: (lambda e: e.max(out=o, in_=i)))(q.t[:, 6, :], selm), [rr], [q.r])
            ix = idx8[b % 2]
            kb.P.op("dve", (lambda o, m_, v: (lambda e: e.max_index(out=o, in_max=m_, in_values=v)))(ix.t[:], q.t[:, 6, :], selm),
                    [rr, q.r], [ix.r])
            kb.cp(q.t[:, 7, :], ix.t[:], [ix.r], [q.r])
            kb.ts(M, selm, q.t[:, 6, 7:8], None, ALU.is_ge, None, [rr, q.r], [rr])
            kb.tt(wn, sc, M, ALU.mult, [rr], [rr])
            s_ = sml[b % 2]
            kb.red(s_.t[:, 0:1], wn, ALU.add, [rr], [s_.r])
            kb.P.op("dve", (lambda o, i: (lambda e: e.reciprocal(out=o, in_=i)))(s_.t[:, 1:2], s_.t[:, 0:1]), [s_.r], [s_.r])
            kb.ts(wn, wn, s_.t[:, 1:2], 2.5, ALU.mult, ALU.mult, [rr, s_.r], [rr])
            kb.cp(S["wn"].t[:, b, :], wn, [rr], [S["wn"].r], eng="act")
        kb.P.flush()


def phase9(kb, io, S):
    c = kb.c
    nc = kb.nc
    T, D, KC, NB, E, CAP, ED, SD = c.T, c.D, c.KC, c.NB, c.E, c.CAP, c.ED, c.SD
    NF = ED // 128
    assert SD == ED
    with ExitStack() as st:
        sb = _sbf(nc, st)
        acc = S["acc"]
        h1T = sb("h1T9", [128, KC, T], BF16)
        kb.dma(h1T.t[:], io["h1T_s"], [], [h1T.r])
        w1 = [sb("w1r%d" % i, [128, KC, ED], BF16) for i in range(2)]
        w3 = [sb("w3r%d" % i, [128, KC, ED], BF16) for i in range(2)]
        w2 = [sb("w2r%d" % i, [128, NF, D], BF16, D // 512) for i in range(1)]
        sl = [sb("slr%d" % i, [128, 512], F32) for i in range(2)]
        hT = sb("hTd", [128, NF, T], BF16)
        wn = S["wn"]

        def load_gu(e):
            a, b3 = w1[(e + 1) % 2], w3[(e + 1) % 2]
            s1 = io["ws1"] if e < 0 else io["w1"][e]
            s3 = io["ws3"] if e < 0 else io["w3"][e]
            kb.dma(a.t[:], s1.rearrange("(kc p) n -> p kc n", p=128), [], [a.r], eng="pool")
            kb.dma(b3.t[:], s3.rearrange("(kc p) n -> p kc n", p=128), [], [b3.r], eng="pool")

        def load_dn(e):
            b2 = w2[0]
            s2 = io["ws2"] if e < 0 else io["w2"][e]
            for ci in range(D // 512):
                kb.dma(b2.t[:, :, ci * 512:(ci + 1) * 512],
                       s2[:, ci * 512:(ci + 1) * 512].rearrange("(f p) n -> p f n", p=128),
                       [], [b2.rs[ci]], eng="pool")

        load_gu(-1)
        load_dn(-1)
        load_gu(0)
        for b in range(NB):
            kb.ts(acc.t[:, b, :], acc.t[:, b, :], c.ALPHA, None, ALU.mult, None, [acc.rs[b]], [acc.rs[b]])
        it = 0
        for e in range(-1, E):
            a, b3, b2 = w1[(e + 1) % 2], w3[(e + 1) % 2], w2[0]
            for fc in range(NF):
                t0 = 0
                while t0 < T:
                    n = min(512, T - t0)
                    s_ = sl[it % 2]
                    p1 = (it % 2) * 2
                    it += 1
                    for kc in range(KC):
                        kb.mm(kb.bank(p1, n), a.t[:, kc, fc * 128:(fc + 1) * 128], h1T.t[:, kc, t0:t0 + n], kc == 0, kc == KC - 1,
                              [a.r, h1T.r], [kb.pr[p1]])
                    for kc in range(KC):
                        kb.mm(kb.bank(p1 + 1, n), b3.t[:, kc, fc * 128:(fc + 1) * 128], h1T.t[:, kc, t0:t0 + n], kc == 0, kc == KC - 1,
                              [b3.r, h1T.r], [kb.pr[p1 + 1]])
                    kb.act(s_.t[:, 0:n], kb.bank(p1, n), AF.Silu, [kb.pr[p1]], [s_.r])
                    kb.tt(hT.t[:, fc, t0:t0 + n], s_.t[:, 0:n], kb.bank(p1 + 1, n), ALU.mult, [s_.r, kb.pr[p1 + 1]], [hT.r])
                    t0 += n
            if e + 2 < E:
                load_gu(e + 2)
            for c0 in range(0, D, 512):
                for b in range(NB):
                    n = min(512, D - c0)
                    pb = 4 + it % 4
                    it += 1
                    for fc in range(NF):
                        kb.mm(kb.bank(pb, n), hT.t[:, fc, b * 128:(b + 1) * 128], b2.t[:, fc, c0:c0 + n], fc == 0, fc == NF - 1,
                              [hT.r, b2.rs[c0 // 512]], [kb.pr[pb]])
                    if e < 0:
                        kb.tt(acc.t[:, b, c0:c0 + n], acc.t[:, b, c0:c0 + n], kb.bank(pb, n), ALU.add, [acc.rs[b], kb.pr[pb]], [acc.rs[b]])
                    else:
                        kb.stt(acc.t[:, b, c0:c0 + n], kb.bank(pb, n), wn.t[:, b, e:e + 1], acc.t[:, b, c0:c0 + n], ALU.mult, ALU.add,
                               [kb.pr[pb], wn.r, acc.rs[b]], [acc.rs[b]])
            if e + 1 < E:
                load_dn(e + 1)
        kb.P.flush()


def phase10(kb, io, S):
    c = kb.c
    nc = kb.nc
    D, NB, E, CAP = c.D, c.NB, c.E, c.CAP
    with ExitStack() as st:
        sb = _sbf(nc, st)
        acc = S["acc"]
        gb = sb("gb2", [128, 2, D], F32)
        kb.dma(gb.t[:, 0, :], io["ln2_g"].to_broadcast([128, D]), [], [gb.r])
        kb.dma(gb.t[:, 1, :], io["ln2_b"].to_broadcast([128, D]), [], [gb.r])
        NH = (D + 1023) // 1024
        sml = [sb("sml2_%d" % i, [128, 4], F32) for i in range(2)]
        jk = sb("jk10", [128, D], F32)
        for b in range(NB):
            _layernorm(kb, sml[b % 2], acc.t[:, b, :], acc.rs[b], acc.t[:, b, :], acc.rs[b], gb, D, c.LN_EPS, jk)
            kb.dma(io["out"][b * 128:(b + 1) * 128, :], acc.t[:, b, :], [acc.rs[b]], [Res("out")])
        kb.P.flush()


def build(cfg, STOP=99):
    c = cfg
    nc = bass.Bass("TRN2", target_bir_lowering=False)

    def din(name, shape, dt=F32):
        return nc.dram_tensor(name, list(shape), dt, kind="ExternalInput").ap()

    def scr(name, shape, dt):
        return nc.dram_tensor(name, list(shape), dt).ap()

    io = {}
    specs = {
        "xTw": [c.D, c.W + 3], "xtok": [c.T, c.D], "valid": [128, c.NW], "memT": [c.D, c.MEM],
        "w_in": [c.D, c.IN], "conv_wT": [128, c.NCC, 4], "conv_bT": [128, c.NCC],
        "dt_bias": [1, c.H], "a_log": [1, c.H], "d_skip": [1, c.H], "ssd_norm_g": [1, c.D],
        "swa_sinks": [1, c.HQ], "swa_bias": [c.HQ, 128, 256], "swa_mask": [2, 128, 256],
        "w_mem_kv": [c.D, 2 * c.XD], "w_ssd_o": [c.D, c.D], "w_swa_o": [c.QD, c.D], "w_xa_o": [c.XD, c.D],
        "w_out": [c.D, c.D], "ln1_g": [1, c.D], "ln1_b": [1, c.D], "router_w": [c.D, c.E], "router_bias": [1, c.E],
        "w1": [c.E, c.D, c.ED], "w3": [c.E, c.D, c.ED], "w2": [c.E, c.ED, c.D],
        "ws1": [c.D, c.SD], "ws3": [c.D, c.SD], "ws2": [c.SD, c.D], "ln2_g": [1, c.D], "ln2_b": [1, c.D],
        "cident": [128, 128], "ctri": [128, 128], "ctris": [128, 128], "cones": [128, 128], "cmneg": [128, 128],
        "ciota": [128, 64], "esel": [c.H, c.H * 128],
    }
    for k, shp in specs.items():
        io[k] = din(k, shp)
    io["out"] = nc.dram_tensor("out", [c.T, c.D], F32, kind="ExternalOutput").ap()
    io["xs_s"] = scr("xs_s", [c.W, c.D], BF16)
    io["B_s"] = scr("B_s", [c.W, 512], BF16)
    io["ynT_s"] = scr("ynT_s", [128, c.KC, c.T], BF16)
    io["oswaT_s"] = scr("oswaT_s", [128, c.QD // 128, c.T], BF16)
    io["oxaT_s"] = scr("oxaT_s", [128, c.XD // 128, c.T], BF16)
    io["mgT_s"] = scr("mgT_s", [128, c.KC, c.T], BF16)
    io["h1T_s"] = scr("h1T_s", [128, c.KC, c.T], BF16)
    NH = (c.D + 1023) // 1024
    io["Xg"] = [scr("Xg%d" % i, [c.E * c.CAP, min(1024, c.D)], BF16) for i in range(NH)]
    io["Yg"] = [scr("Yg%d" % i, [c.E * c.CAP, min(1024, c.D)], F32) for i in range(NH)]
    with ExitStack() as es:
        kb = KB(nc, c, es)
        sb = _sbf(nc, es)
        S = {}
        S["ident"] = sb("ident", [128, 128], BF16)
        kb.dma(S["ident"].t[:], io["cident"], [], [S["ident"].r], eng="pool")
        for nm, src in (("identf", "cident"), ("tri", "ctri"), ("tris", "ctris"), ("ones", "cones"), ("mneg", "cmneg")):
            S[nm] = sb(nm, [128, 128], F32)
            kb.dma(S[nm].t[:], io[src], [], [S[nm].r])
        S["iota"] = sb("iota", [128, 64], F32)
        kb.dma(S["iota"].t[:], io["ciota"], [], [S["iota"].r])
        with ExitStack() as s1:
            sb1 = _sbf(nc, s1)
            S["BT"] = sb1("BT", [128, 4, c.T], BF16, 4)
            S["CT"] = sb1("CT", [128, 4, c.T], BF16, 4)
            S["dt"] = sb1("dt", [128, c.NW, c.H], F32)
            S["la"] = sb1("la", [128, c.NW, c.H], F32)
            S["ytok"] = sb1("ytok", [128, c.NB, c.D], BF16, c.NB)
            S["xs_res"] = [Res("xs%d" % i) for i in range(c.W // 256)]
            S["B_res"] = [Res("Bs%d" % i) for i in range(c.W // 256)]
            phase1(kb, io, S)
            if STOP >= 2:
                phase2(kb, io, S)
            if STOP >= 3:
                phase3(kb, io, S)
        if STOP >= 4:
            phase4(kb, io, S)
        if STOP >= 5:
            phase5(kb, io, S)
        if STOP >= 6:
            phase6(kb, io, S)
        with ExitStack() as s2:
            if STOP < 7:
                return nc
            sb2 = _sbf(nc, s2)
            S["acc"] = sb2("accum", [128, c.NB, c.D], F32, c.NB)
            S["dint"] = sb2("dint", [128, c.NB, 8], I32, c.NB)
            S["wk"] = sb2("wk", [128, c.NB, 8], F32)
            S["wn"] = sb2("wn", [128, c.NB, c.E], F32)
            phase7(kb, io, S)
            if STOP >= 9:
                phase9(kb, io, S)
            if STOP >= 10:
                phase10(kb, io, S)
        print("bass ops:", kb.P.nops)
    return nc


def t5_bucket(dist):
    dist = np.asarray(dist)
    max_exact = 16
    d = np.maximum(dist, 1).astype(np.float32)
    large = max_exact + (np.log(d / np.float32(max_exact)) / np.float32(math.log(128 / max_exact)) * np.float32(32 - max_exact)).astype(np.int32)
    large = np.minimum(large, 31)
    return np.where(dist < max_exact, dist, large)


def host_constants(cfg):
    c = cfg
    j = np.arange(128)[:, None]
    i = np.arange(128)[None, :]
    k = {}
    k["cident"] = (j == i).astype(np.float32)
    k["ctri"] = (j <= i).astype(np.float32)
    k["ctris"] = (j < i).astype(np.float32)
    k["cones"] = np.ones((128, 128), np.float32)
    k["cmneg"] = np.where(j <= i, 0.0, NEG).astype(np.float32)
    k["ciota"] = np.tile(np.arange(64, dtype=np.float32)[None, :], (128, 1))
    es = np.zeros((c.H, c.H, 128), np.float32)
    for h in range(c.H):
        es[h, h, :] = 1.0
    k["esel"] = es.reshape(c.H, c.H * 128)
    return k


def prep_core(cfg, inp, core, consts):
    c = cfg
    b = core // c.CPS
    s0 = (core % c.CPS) * c.T
    x = inp["x"]
    m = {}
    lo = s0 + c.T - c.W - 3
    xw = np.zeros((c.W + 3, c.D), np.float32)
    src_lo = max(lo, 0)
    xw[src_lo - lo:, :] = x[b, src_lo:s0 + c.T, :]
    m["xTw"] = np.ascontiguousarray(xw.T)
    m["xtok"] = np.ascontiguousarray(x[b, s0:s0 + c.T, :])
    pos = s0 + c.T - c.W + np.arange(c.W)
    m["valid"] = np.ascontiguousarray((pos >= 0).astype(np.float32).reshape(c.NW, 128).T)
    m["memT"] = np.ascontiguousarray(inp["mem"][b].T)
    m["w_in"] = inp["w_in"][0]
    cwv = inp["conv_w"][0]
    m["conv_wT"] = np.ascontiguousarray(cwv.T.reshape(c.NCC, 128, 4).transpose(1, 0, 2))
    m["conv_bT"] = np.ascontiguousarray(inp["conv_b"][0].reshape(c.NCC, 128).T)
    for nm in ("dt_bias", "a_log", "d_skip", "ssd_norm_g", "swa_sinks", "ln1_g", "ln1_b", "router_bias", "ln2_g", "ln2_b"):
        m[nm] = np.ascontiguousarray(inp[nm][0].reshape(1, -1))
    qi = np.arange(128)[:, None]
    kj = np.arange(256)[None, :]
    dist = qi + 128 - kj
    bias = inp["rel_bias"][t5_bucket(np.maximum(dist, 0))]
    m["swa_bias"] = np.ascontiguousarray(bias.transpose(2, 0, 1))
    inwin = (dist >= 0) & (dist < 128)
    mrest = np.where(inwin, 0.0, NEG).astype(np.float32)
    mfirst = np.where(inwin & (kj >= 128), 0.0, NEG).astype(np.float32)
    m["swa_mask"] = np.stack([mfirst if s0 == 0 else mrest, mrest]).astype(np.float32)
    for nm in ("w_mem_kv", "w_ssd_o", "w_swa_o", "w_xa_o", "w_out", "router_w", "w1", "w3", "w2", "ws1", "ws3", "ws2"):
        m[nm] = inp[nm][0]
    m.update(consts)
    return m


_NC_CACHE = {}


def kernel(**inputs):
    cfg = Cfg()
    inp = {k: np.asarray(v) for k, v in inputs.items()}
    if "nc" not in _NC_CACHE:
        _NC_CACHE["nc"] = build(cfg)
    nc = _NC_CACHE["nc"]
    consts = host_constants(cfg)
    in_maps = [prep_core(cfg, inp, core, consts) for core in range(cfg.NCORES)]
    res = run_bass_kernel_spmd(nc, in_maps, core_ids=list(range(cfg.NCORES)))
    out = np.zeros((cfg.BATCH, cfg.CPS * cfg.T, cfg.D), np.float32)
    for core in range(cfg.NCORES):
        b = core // cfg.CPS
        s0 = (core % cfg.CPS) * cfg.T
        out[b, s0:s0 + cfg.T, :] = res.results[core]["out"]
    return out
```
